# Optimizing a Trainium2 kernel written in Bass

```python
import jax, jax.numpy as jnp
from jax import lax
import numpy as np

D_MODEL = 1024
BATCH = 4
SEQ = 4096
DEPTH = 4

RET_HEADS = 4
RET_DK = 128
RET_DV = 128
FOX_HEADS = 8
FOX_DH = 64
D_FF = 2816
CHUNK = 128
Q_BLOCK = 128
ROPE_BASE = 10000.0
EPS = 1e-6

RET_QK = RET_HEADS * RET_DK
RET_V = RET_HEADS * RET_DV
FOX_W = FOX_HEADS * FOX_DH
IN_SPLITS = (RET_QK, RET_QK, RET_V, RET_V, FOX_W, FOX_W, FOX_W, FOX_HEADS, D_MODEL, D_MODEL)
IN_COLS = sum(IN_SPLITS)

kernel_name = "hybrid_retention_forgetting_attention_macaron"


def rmsnorm(x, g):
    xf = x.astype(jnp.float32)
    y = xf * lax.rsqrt(jnp.mean(xf * xf, axis=-1, keepdims=True) + EPS)
    return (y * g.astype(jnp.float32)).astype(x.dtype)


def swiglu(h, w_in, w_out):
    gu = h @ w_in
    a, b = gu[..., :D_FF], gu[..., D_FF:]
    return (jax.nn.silu(a) * b) @ w_out


def split_cols(p):
    outs = []
    off = 0
    for w in IN_SPLITS:
        outs.append(p[..., off:off + w])
        off += w
    return outs


def to_heads(t, n_heads):
    b, s, _ = t.shape
    return t.reshape(b, s, n_heads, -1).transpose(0, 2, 1, 3)


def from_heads(t):
    b, h, s, d = t.shape
    return t.transpose(0, 2, 1, 3).reshape(b, s, h * d)


def rope(t):
    s, d = t.shape[2], t.shape[3]
    inv = jnp.power(ROPE_BASE, -jnp.arange(0, d, 2, dtype=jnp.float32) / d)
    ang = jnp.arange(s, dtype=jnp.float32)[:, None] * inv[None, :]
    cos, sin = jnp.cos(ang), jnp.sin(ang)
    tf = t.astype(jnp.float32)
    t1, t2 = tf[..., : d // 2], tf[..., d // 2:]
    return jnp.concatenate([t1 * cos - t2 * sin, t1 * sin + t2 * cos], axis=-1).astype(t.dtype)


def retention_chunkwise(q, k, v):
    in_dtype = v.dtype
    q = q.astype(jnp.float32)
    k = k.astype(jnp.float32) * (q.shape[-1] ** -0.5)
    v = v.astype(jnp.float32)
    b, h, s, dk = q.shape
    dv = v.shape[-1]
    n = s // CHUNK
    log_gamma = jnp.log1p(-jnp.exp2(-5.0 - jnp.arange(h, dtype=jnp.float32)))
    idx = jnp.arange(CHUNK, dtype=jnp.float32)
    diff = idx[:, None] - idx[None, :]
    decay_intra = jnp.where(diff >= 0, jnp.exp(log_gamma[:, None, None] * jnp.maximum(diff, 0.0)), 0.0)
    xi = jnp.exp(log_gamma[:, None] * (idx + 1.0))
    zeta = jnp.exp(log_gamma[:, None] * (CHUNK - 1.0 - idx))
    gamma_c = jnp.exp(log_gamma * CHUNK)

    qc = q.reshape(b, h, n, CHUNK, dk)
    kc = k.reshape(b, h, n, CHUNK, dk)
    vc = v.reshape(b, h, n, CHUNK, dv)

    scores = jnp.einsum('bhncd,bhnsd->bhncs', qc, kc) * decay_intra[None, :, None]
    inner = jnp.einsum('bhncs,bhnse->bhnce', scores, vc)

    kv = jnp.einsum('bhncd,bhnce->bhnde', kc * zeta[None, :, None, :, None], vc)

    def step(state, kv_n):
        return gamma_c[None, :, None, None] * state + kv_n, state

    _, state_prev = lax.scan(step, jnp.zeros((b, h, dk, dv), jnp.float32), jnp.moveaxis(kv, 2, 0))
    state_prev = jnp.moveaxis(state_prev, 0, 2)
    cross = jnp.einsum('bhncd,bhnde->bhnce', qc * xi[None, :, None, :, None], state_prev)
    return (inner + cross).reshape(b, h, s, dv).astype(in_dtype)


def forgetting_attention(q, k, v, log_f):
    b, h, s, d = q.shape
    c = jnp.cumsum(log_f, axis=-1)
    kpos = jnp.arange(s)
    scale = d ** -0.5
    n_blocks = s // Q_BLOCK

    def block(i):
        start = i * Q_BLOCK
        qb = lax.dynamic_slice_in_dim(q, start, Q_BLOCK, axis=2)
        cb = lax.dynamic_slice_in_dim(c, start, Q_BLOCK, axis=2)
        qpos = start + jnp.arange(Q_BLOCK)
        logits = jnp.einsum('bhqd,bhkd->bhqk', qb, k).astype(jnp.float32) * scale
        logits = logits + (cb[..., :, None] - c[..., None, :])
        logits = jnp.where(kpos[None, :] <= qpos[:, None], logits, -1e30)
        p = jax.nn.softmax(logits, axis=-1)
        return jnp.einsum('bhqk,bhkd->bhqd', p.astype(v.dtype), v)

    out = lax.map(block, jnp.arange(n_blocks))
    return jnp.moveaxis(out, 0, 2).reshape(b, h, s, d)


def mixing_sublayer(h, w_in, b_forget, ret_norm, w_o_ret, w_o_fox, w_out):
    proj = h @ w_in
    rq, rk, rv, rg, fq, fk, fv, ff, gate_r, gate_f = split_cols(proj)

    ret = retention_chunkwise(rope(to_heads(rq, RET_HEADS)), rope(to_heads(rk, RET_HEADS)), to_heads(rv, RET_HEADS))
    retf = ret.astype(jnp.float32)
    retf = retf * lax.rsqrt(jnp.mean(retf * retf, axis=-1, keepdims=True) + EPS)
    ret = from_heads(retf.astype(h.dtype)) * ret_norm
    y_ret = (jax.nn.silu(rg) * ret) @ w_o_ret

    log_f = jax.nn.log_sigmoid((ff + b_forget).astype(jnp.float32))
    fox = forgetting_attention(to_heads(fq, FOX_HEADS), to_heads(fk, FOX_HEADS), to_heads(fv, FOX_HEADS),
                               log_f.transpose(0, 2, 1))
    y_fox = from_heads(fox) @ w_o_fox

    merged = jax.nn.sigmoid(gate_r) * y_ret + jax.nn.sigmoid(gate_f) * y_fox
    return merged @ w_out


def setup_inputs(seed: int = 0) -> dict:
    key = jax.random.key(seed)
    ks = jax.random.split(key, 16)
    f32 = jnp.float32

    def nrm(k, shape, fan_in):
        return jax.random.normal(k, shape, f32) * (fan_in ** -0.5)

    def gain(k, shape):
        return 1.0 + 0.02 * jax.random.normal(k, shape, f32)

    return {
        "x": jax.random.normal(ks[0], (BATCH, SEQ, D_MODEL), f32),
        "norm_ffn1": gain(ks[1], (DEPTH, D_MODEL)),
        "w_ffn1_in": nrm(ks[2], (DEPTH, D_MODEL, 2 * D_FF), D_MODEL),
        "w_ffn1_out": nrm(ks[3], (DEPTH, D_FF, D_MODEL), D_FF),
        "norm_mix": gain(ks[4], (DEPTH, D_MODEL)),
        "w_in": nrm(ks[5], (DEPTH, D_MODEL, IN_COLS), D_MODEL),
        "b_forget": jax.random.uniform(ks[6], (DEPTH, FOX_HEADS), f32, minval=1.0, maxval=4.0),
        "ret_norm": gain(ks[7], (DEPTH, RET_V)),
        "w_o_ret": nrm(ks[8], (DEPTH, RET_V, D_MODEL), RET_V),
        "w_o_fox": nrm(ks[9], (DEPTH, FOX_W, D_MODEL), FOX_W),
        "w_out": nrm(ks[10], (DEPTH, D_MODEL, D_MODEL), D_MODEL),
        "norm_ffn2": gain(ks[11], (DEPTH, D_MODEL)),
        "w_ffn2_in": nrm(ks[12], (DEPTH, D_MODEL, 2 * D_FF), D_MODEL),
        "w_ffn2_out": nrm(ks[13], (DEPTH, D_FF, D_MODEL), D_FF),
        "norm_final": gain(ks[14], (D_MODEL,)),
    }


def reference(x, norm_ffn1, w_ffn1_in, w_ffn1_out, norm_mix, w_in, b_forget, ret_norm,
              w_o_ret, w_o_fox, w_out, norm_ffn2, w_ffn2_in, w_ffn2_out, norm_final):
    for l in range(DEPTH):
        x = x + 0.5 * swiglu(rmsnorm(x, norm_ffn1[l]), w_ffn1_in[l], w_ffn1_out[l])
        h = rmsnorm(x, norm_mix[l])
        x = x + mixing_sublayer(h, w_in[l], b_forget[l], ret_norm[l], w_o_ret[l], w_o_fox[l], w_out[l])
        x = x + 0.5 * swiglu(rmsnorm(x, norm_ffn2[l]), w_ffn2_in[l], w_ffn2_out[l])
    return rmsnorm(x, norm_final)
```

```python
import numpy as np
from contextlib import ExitStack
import concourse.bass as bass
import concourse.mybir as mybir
from concourse.bass_utils import run_bass_kernel_spmd

F32 = mybir.dt.float32
BF16 = mybir.dt.bfloat16
AF = mybir.ActivationFunctionType
ALU = mybir.AluOpType
AX = mybir.AxisListType

D = 1024
NBATCH = 4
SEQ = 4096
DEPTH = 4
T = 2048
NT = 4
NB = 16
DFF = 2816
EPS = 1e-6
TPL = 51
NCF = 1024
GAMMA = [1.0 - 2.0 ** (-5 - h) for h in range(4)]


class Buf:
    __slots__ = ("name", "lw", "rd", "dsem", "dcnt", "dlast")

    def __init__(self, name):
        self.name = name
        self.lw = None
        self.rd = {}
        self.dsem = None
        self.dcnt = 0
        self.dlast = None


class Sched:
    ENGS = ("pe", "act", "dve", "pool", "sp")
    EPOCH = 30000

    def __init__(self, nc):
        self.nc = nc
        self.lists = {e: [] for e in self.ENGS}
        self.cnt = {e: 0 for e in self.ENGS}
        self.nsem = 0
        self.sem = {e: self._new_sem(e) for e in self.ENGS}
        self.waited = {e: {} for e in self.ENGS}
        self.last = {e: None for e in self.ENGS}
        self.dmabufs = []
        self.nops = 0

    def _new_sem(self, tag):
        self.nsem += 1
        return self.nc.alloc_semaphore(name=f"s{self.nsem}_{tag}")

    def _wait(self, eng, waits, t):
        sem, val = t[0], t[1]
        wd = self.waited[eng]
        k = id(sem)
        if wd.get(k, 0) >= val:
            return
        wd[k] = val
        waits.append((sem, val))

    def op(self, eng, fn, reads=(), writes=(), dma=None, cc=False):
        waits = []
        deps = []
        for b in reads:
            if b.lw is not None:
                deps.append((b.lw, 0))
        for b in writes:
            if b.lw is not None:
                deps.append((b.lw, 0))
            for t in b.rd.values():
                deps.append((t, 1))
        if dma is not None and dma.dlast is not None:
            deps.append((dma.dlast, 0))
        for (t, war) in deps:
            if (not t[3]) and t[2] == eng and eng == "pe":
                continue
            self._wait(eng, waits, t)
        if dma is not None:
            if dma.dsem is None or dma.dcnt >= 48000 or cc:
                dma.dsem = self._new_sem("d")
                dma.dcnt = 0
                if dma not in self.dmabufs:
                    self.dmabufs.append(dma)
            inc = 1 if cc else 16
            dma.dcnt += inc
            ticket = (dma.dsem, dma.dcnt, eng, True)
            dma.dlast = ticket
        else:
            if self.cnt[eng] >= self.EPOCH:
                self.sem[eng] = self._new_sem(eng)
                self.cnt[eng] = 0
            self.cnt[eng] += 1
            ticket = (self.sem[eng], self.cnt[eng], eng, False)
            self.last[eng] = ticket
            inc = 1
        self.lists[eng].append((waits, fn, ticket[0], inc, cc))
        self.nops += 1
        for b in writes:
            b.lw = ticket
            b.rd = {}
        for b in reads:
            if b.lw is not ticket:
                b.rd[id(ticket[0])] = ticket
        return ticket

    def barrier(self, skip=()):
        ts = [self.last[e] for e in self.ENGS if self.last[e] is not None]
        for b in self.dmabufs:
            if b.dlast is not None and b not in skip:
                ts.append(b.dlast)
        for e in self.ENGS:
            waits = []
            for t in ts:
                if (not t[3]) and t[2] == e:
                    continue
                self._wait(e, waits, t)
            if waits:
                self.lists[e].append((waits, None, None, 0, False))

    def emit(self):
        nc = self.nc
        with nc.Block() as block:
            def mk(ename):
                lst = self.lists[ename]

                def body(e):
                    for (waits, fn, sem, inc, cc) in lst:
                        for (s, v) in waits:
                            e.wait_ge(s, v)
                        if fn is not None:
                            ins = fn(e)
                            if cc:
                                ins.then_inc(sem)
                            else:
                                ins.then_inc(sem, inc)
                return body
            block.tensor(mk("pe"))
            block.scalar(mk("act"))
            block.vector(mk("dve"))
            block.gpsimd(mk("pool"))
            block.sync(mk("sp"))


def build_nc(nl=DEPTH, stop=None):
    nc = bass.Bass("TRN2", target_bir_lowering=False)
    es = ExitStack()
    S = Sched(nc)

    def din(name, shape, dt=F32):
        return nc.dram_tensor(name, list(shape), dt, kind="ExternalInput").ap()

    xT_in = din("xT", [D, T])
    wseq = din("wseq", [nl * TPL, 128, 4096])
    ffw = din("ffw", [nl, 128, 64])
    c_f32 = din("c_f32", [128, 2176])
    c_rope = din("c_rope", [4, 128, T])
    outT = nc.dram_tensor("outT", [D, T], F32, kind="ExternalOutput").ap()
    EXK = nc.dram_tensor("exk", [128, 8192], BF16)
    GXK = nc.dram_tensor("gxk", [256, 8192], BF16)
    EXV = nc.dram_tensor("exv", [128, 8192], BF16)
    GXV = nc.dram_tensor("gxv", [256, 8192], BF16)
    EXF = nc.dram_tensor("exf", [128, NCF], F32)
    GXF = nc.dram_tensor("gxf", [256, NCF], F32)
    LKZ = nc.dram_tensor("lkz", [128, 8192], BF16)
    LRV = nc.dram_tensor("lrv", [128, 8192], BF16)
    exk, gxk, exv, gxv, exf, gxf, lkz, lrv = EXK.ap(), GXK.ap(), EXV.ap(), GXV.ap(), EXF.ap(), GXF.ap(), LKZ.ap(), LRV.ap()
    bGXK, bGXV, bEXF, bGXF = Buf("gxk"), Buf("gxv"), Buf("exf"), Buf("gxf")
    pieces = {}

    def pc_(name):
        if name not in pieces:
            pieces[name] = Buf(name)
        return pieces[name]

    NCOL = 104448
    REG = es.enter_context(nc.sbuf_tensor("reg", [128, NCOL], BF16))
    PSB = [es.enter_context(nc.psum_tensor(f"psb{i}", [128, 512], F32)) for i in range(7)]
    PST = es.enter_context(nc.psum_tensor("pst", [128, 1024], BF16))

    def carve(off, ncols, dt=BF16, **rr):
        ap = REG[:, off:off + ncols]
        if dt == F32:
            ap = ap.bitcast(F32)
        return ap

    O_XT, O_HT, O_WR, O_RB, O_RC, O_RD, O_RE, O_RS = 0, 32768, 49152, 61440, 69632, 77824, 86144, 90240
    XT = carve(O_XT, 32768, F32).rearrange("p (c t) -> p c t", c=8)
    HT = carve(O_HT, 16384).rearrange("p (c t) -> p c t", c=8)
    XTb = [[Buf(f"xt{c}_{t}") for t in range(NT)] for c in range(8)]
    HTb = [[Buf(f"ht{c}_{t}") for t in range(NT)] for c in range(8)]
    WSL = [carve(O_WR + i * 4096, 4096) for i in range(3)]
    WSLb = [Buf(f"w{i}") for i in range(3)]
    RB = carve(O_RB, 8192).rearrange("p (c t) -> p c t", c=4)
    RBb = [[Buf(f"rb{c}_{t}") for t in range(NT)] for c in range(4)]
    RC = carve(O_RC, 8192).rearrange("p (c t) -> p c t", c=4)
    RCb = [[Buf(f"rc{c}_{t}") for t in range(NT)] for c in range(4)]

    rs_off = [O_RS]

    def rs(ncols, dt=BF16):
        o = rs_off[0]
        rs_off[0] += ncols
        assert rs_off[0] <= NCOL, rs_off[0]
        return carve(o, ncols, dt)

    CF = rs(4352, F32)
    bCF = Buf("cf")
    c_identf = CF[:, 0:128]
    c_onesf = CF[:, 128:256]
    c_tric = CF[:, 256:384]
    c_decm = CF[:, 384:896].rearrange("p (h c) -> p h c", h=4)
    c_xi = CF[:, 896:1408].rearrange("p (h c) -> p h c", h=4)
    c_zeta = CF[:, 1408:1412]
    c_kdec = CF[:, 1412:1476].rearrange("p (j h) -> p j h", j=16)
    c_fl = CF[:, 1476:1480]
    c_gn = CF[:, 1480:1584]
    c_rnw = CF[:, 1584:1600]
    c_bf = CF[:, 1600:1632]
    c_tri = CF[:, 1632:1760]
    c_ma = CF[:, 1760:1888]
    c_sel2 = CF[:, 1888:2144].rearrange("p (a m) -> p a m", a=2)
    IDB = rs(128); ONB = rs(128); TRIB = rs(128); MAB = rs(128)
    SEL2B = rs(256).rearrange("p (a m) -> p a m", a=2)
    bIDB, bONB, bTRIB, bMAB, bSEL = Buf("idb"), Buf("onb"), Buf("trib"), Buf("mab"), Buf("sel")
    WFF = rs(64)
    bWFF = Buf("wff")

    S.op("sp", lambda e: e.dma_start(out=CF, in_=c_f32), writes=[bCF], dma=bCF)
    for (dst, src, b) in ((IDB, c_identf, bIDB), (ONB, c_onesf, bONB), (TRIB, c_tri, bTRIB), (MAB, c_ma, bMAB)):
        S.op("dve", lambda e, dst=dst, src=src: e.tensor_copy(out=dst, in_=src), reads=[bCF], writes=[b])
    S.op("dve", lambda e: e.tensor_copy(out=SEL2B, in_=c_sel2), reads=[bCF], writes=[bSEL])
    xv = xT_in.rearrange("(c p) t -> p c t", p=128)
    bx = Buf("xload")
    for c in range(8):
        S.op("sp", lambda e, c=c: e.dma_start(out=XT[:, c, :], in_=xv[:, c, :]), writes=XTb[c], dma=bx)

    psbig_b = [Buf(f"psbig{i}") for i in range(3)]
    psbig_i = [0]

    def psbig():
        i = psbig_i[0] % 3
        psbig_i[0] += 1
        return PSB[i], psbig_b[i]

    def mkrot(banks, name):
        bufs = [Buf(f"{name}{i}") for i in range(len(banks))]
        idx = [0]

        def get():
            i = idx[0] % len(banks)
            idx[0] += 1
            return banks[i][:, 0:128], bufs[i]
        get.bufs = bufs
        return get
    ps4 = mkrot([PSB[3], PSB[4]], "ps4")
    ps5 = mkrot([PSB[5]], "ps5")
    ps6 = mkrot([PSB[6]], "ps6")
    pst = mkrot([PST], "pst")
    ps7_banks = [(PSB[0], psbig_b[0]), (PSB[1], psbig_b[1]), (PSB[2], psbig_b[2]), (PSB[3], ps4.bufs[0]), (PSB[4], ps4.bufs[1]),
                 (PSB[5], ps5.bufs[0]), (PSB[6], ps6.bufs[0])]
    ps7_i = [0]

    def ps7():
        i = ps7_i[0] % 7
        ps7_i[0] += 1
        return ps7_banks[i]

    wstate = {"next_dma": 0, "next_get": 0, "total": nl * TPL}

    def wget(k=1):
        f = wstate["next_get"]
        lim = min(f + 3, wstate["total"])
        while wstate["next_dma"] < lim:
            i = wstate["next_dma"]
            sl = i % 3
            S.op("pool", lambda e, i=i, sl=sl: e.dma_start(out=WSL[sl], in_=wseq[i]), writes=[WSLb[sl]], dma=WSLb[sl])
            wstate["next_dma"] += 1
        wstate["next_get"] += k
        return [(WSL[(f + i) % 3], WSLb[(f + i) % 3]) for i in range(k)]

    def tsl(t):
        return slice(t * 512, (t + 1) * 512)

    def mm(ps, psb, lhsT, rhs, rds, start, stop):
        S.op("pe", lambda e: e.matmul(ps, lhsT=lhsT, rhs=rhs, start=start, stop=stop), reads=rds, writes=[psb])

    def rmsnorm(gcol, final=False):
        sq = carve(O_RE, 4096).rearrange("p (c t) -> p c t", c=8)
        sd = carve(O_RD, 1024, F32)
        rstd = carve(O_RD + 1024, 1024, F32)
        bsq, bsd, brs = Buf("sq"), Buf("sd"), Buf("rstd")
        for t in range(NT):
            xr = [XTb[c][t] for c in range(8)]
            S.op("act", lambda e, t=t: e.activation(out=sq, in_=XT[:, :, tsl(t)], func=AF.Square), reads=xr, writes=[bsq])
            ps, pb = psbig()
            for c in range(8):
                mm(ps[:, :], pb, ONB, sq[:, c, :], [bONB, bsq], c == 0, c == 7)
            S.op("act", lambda e, ps=ps: e.activation(out=sd, in_=ps[:, :], func=AF.Sqrt, scale=1.0 / D, bias=EPS), reads=[pb], writes=[bsd])
            S.op("dve", lambda e: e.reciprocal(out=rstd, in_=sd), reads=[bsd], writes=[brs])
            for c in range(8):
                if final:
                    S.op("dve", lambda e, c=c, t=t: e.scalar_tensor_tensor(out=XT[:, c, tsl(t)], in0=XT[:, c, tsl(t)], scalar=c_gn[:, gcol + c:gcol + c + 1], in1=rstd, op0=ALU.mult, op1=ALU.mult),
                         reads=[XTb[c][t], brs, bCF], writes=[XTb[c][t]])
                else:
                    S.op("dve", lambda e, c=c, t=t: e.scalar_tensor_tensor(out=HT[:, c, tsl(t)], in0=XT[:, c, tsl(t)], scalar=c_gn[:, gcol + c:gcol + c + 1], in1=rstd, op0=ALU.mult, op1=ALU.mult),
                         reads=[XTb[c][t], brs, bCF], writes=[HTb[c][t]])

    def ffn(l, which):
        S.barrier(skip=WSLb)
        rmsnorm((l * 3 + (0 if which == 0 else 2)) * 8)
        S.barrier(skip=WSLb)
        actT, actb = RB, RBb
        sl_t = [carve(O_RC + i * 1024, 1024, F32) for i in range(4)]
        sl_b = [Buf(f"slt{i}") for i in range(4)]
        k = 0
        for gi in range(6):
            nch = 4 if gi < 5 else 2
            for ip in range(nch // 2):
                (w, wb), = wget(1)
                wv = w.rearrange("p (c k n) -> p c k n", c=4, k=8)
                for ci in range(2):
                    lc = ip * 2 + ci
                    for t in range(NT):
                        pa, pab = ps7()
                        pbm, pbb = ps7()
                        for kc in range(8):
                            mm(pa[:, :], pab, wv[:, ci, kc, :], HT[:, kc, tsl(t)], [wb, HTb[kc][t]], kc == 0, kc == 7)
                        for kc in range(8):
                            mm(pbm[:, :], pbb, wv[:, 2 + ci, kc, :], HT[:, kc, tsl(t)], [wb, HTb[kc][t]], kc == 0, kc == 7)
                        st, sb = sl_t[k % 4], sl_b[k % 4]
                        k += 1
                        S.op("act", lambda e, pa=pa, st=st: e.activation(out=st, in_=pa[:, :], func=AF.Silu), reads=[pab], writes=[sb])
                        S.op("dve", lambda e, pbm=pbm, st=st, lc=lc, t=t: e.tensor_tensor(out=actT[:, lc, tsl(t)], in0=pbm[:, :], in1=st, op=ALU.mult),
                             reads=[pbb, sb], writes=[actb[lc][t]])
            (wo, wob), = wget(1)
            wov = wo.rearrange("p (c n) -> p c n", c=4)
            for o in range(8):
                for t in range(NT):
                    ps, pb = ps7()
                    for lc in range(nch):
                        mm(ps[:, :], pb, wov[:, lc, o * 128:(o + 1) * 128], actT[:, lc, tsl(t)], [wob, actb[lc][t]], lc == 0, lc == nch - 1)
                    S.op("dve", lambda e, ps=ps, o=o, t=t: e.scalar_tensor_tensor(out=XT[:, o, tsl(t)], in0=ps[:, :], scalar=0.5, in1=XT[:, o, tsl(t)], op0=ALU.mult, op1=ALU.add),
                         reads=[pb, XTb[o][t]], writes=[XTb[o][t]])

    def proj_fm(wv, wb, cc, t, ps, pb):
        for kc in range(8):
            mm(ps, pb, wv[:, cc, kc, :], HT[:, kc, tsl(t)], [wb, HTb[kc][t]], kc == 0, kc == 7)

    def mixer(l):
        S.barrier(skip=WSLb)
        rmsnorm((l * 3 + 1) * 8)
        S.barrier(skip=WSLb)
        scale = 0.125
        tabs = [[carve(O_RD + (i * 2 + j) * 1024, 1024, F32) for j in range(2)] for i in range(2)]
        tabb = [Buf("tab0"), Buf("tab1")]
        t12 = [[carve(O_RD + 4096 + (i * 2 + j) * 1024, 1024, F32) for j in range(2)] for i in range(2)]
        t12b = [[Buf(f"t12_{i}{j}") for j in range(2)] for i in range(2)]
        stg = [carve(O_RE + i * 512, 512) for i in range(2)]
        stgb = [Buf("stg0"), Buf("stg1")]
        kst = [[carve(O_RE + 1024 + (i * 2 + j) * 128, 128) for j in range(2)] for i in range(2)]
        kstb = [[Buf(f"kst{i}{j}") for j in range(2)] for i in range(2)]
        qsq = carve(O_RE + 1536, 512)
        bqsq = Buf("qsq")
        sm = mixer.small
        krT, krb = RB, RBb
        qxiT, qxb = RC, RCb
        S.op("pool", lambda e: e.memset(sm["kmax"], 0.0), writes=[sm["bkmax"]])

        def norm2max(ps, pb, cc, t, acc, accb):
            S.op("act", lambda e: e.activation(out=qsq, in_=ps, func=AF.Square), reads=[pb], writes=[bqsq])
            for hh in range(2):
                p2, p2b = psbig()
                mm(p2[:, :], p2b, SEL2B[:, hh, :], qsq, [bSEL, bqsq], True, True)
                S.op("dve", lambda e, p2=p2: e.reduce_max(out=sm["red"], in_=p2[:, :], axis=AX.X), reads=[p2b], writes=[sm["bred"]])
                col = cc * 2 + hh
                S.op("dve", lambda e, col=col: e.tensor_tensor(out=acc[:, col:col + 1], in0=acc[:, col:col + 1], in1=sm["red"], op=ALU.max),
                     reads=[sm["bred"], accb], writes=[accb])

        (w, wb), = wget(1)
        wv = w.rearrange("p (c k n) -> p c k n", c=4, k=8)
        k = 0
        for cc in range(4):
            for t in range(NT):
                ps, pb = psbig()
                proj_fm(wv, wb, cc, t, ps[:, :], pb)
                st, sb = stg[k % 2], stgb[k % 2]
                k += 1
                S.op("act", lambda e, ps=ps, st=st: e.activation(out=st, in_=ps[:, :], func=AF.Copy), reads=[pb], writes=[sb])
                S.op("sp", lambda e, st=st, cc=cc, t=t: e.dma_start(out=exk[:, cc * T + t * 512: cc * T + (t + 1) * 512], in_=st),
                     reads=[sb], writes=[pc_(f"fk{cc}_{t}")], dma=sb)
                norm2max(ps[:, :], pb, cc, t, sm["kmax"], sm["bkmax"])
        rg_ = [[0, 1], [2, 3], [4, 5], [6, 7]]
        S.op("pool", lambda e: e.collective_compute("AllGather", ALU.bypass, replica_groups=rg_, ins=[EXK.ap().opt()], outs=[GXK.ap().opt()]),
             reads=[b for n, b in pieces.items() if n.startswith("fk")] + [bGXF], writes=[bGXK], dma=bGXK, cc=True)
        if stop == "m2a":
            return
        (w, wb), = wget(1)
        wv = w.rearrange("p (k n) -> p k n", k=8)
        exv_v = exv.rearrange("p (h j e) -> p h j e", h=8, j=16)
        for blk in range(NB):
            t = blk // 4
            ps, pb = psbig()
            for kc in range(8):
                mm(ps[:, :], pb, HT[:, kc, blk * 128:(blk + 1) * 128], wv[:, kc, :], [wb, HTb[kc][t]], kc == 0, kc == 7)
            st, sb = stg[blk % 2], stgb[blk % 2]
            S.op("act", lambda e, ps=ps, st=st: e.activation(out=st, in_=ps[:, :], func=AF.Copy), reads=[pb], writes=[sb])
            S.op("sp", lambda e, st=st, blk=blk: e.dma_start(out=exv_v[:, :, blk, :], in_=st.rearrange("p (h e) -> p h e", h=8)), reads=[sb], writes=[pc_(f"fv{blk}")], dma=sb)
        S.op("pool", lambda e: e.collective_compute("AllGather", ALU.bypass, replica_groups=rg_, ins=[EXV.ap().opt()], outs=[GXV.ap().opt()]),
             reads=[b for n, b in pieces.items() if n.startswith("fv")] + [bGXK], writes=[bGXV], dma=bGXV, cc=True)
        if stop == "m2b":
            return
        (w, wb), = wget(1)
        wv = w.rearrange("p (k n) -> p k n", k=8)
        rvall = carve(O_RC, 8192).rearrange("p (j n) -> p j n", j=16)
        lrv_v = lrv.rearrange("p (h j d) -> p h j d", h=4, j=16)
        for blk in range(NB):
            t = blk // 4
            ps, pb = psbig()
            for kc in range(8):
                mm(ps[:, :], pb, HT[:, kc, blk * 128:(blk + 1) * 128], wv[:, kc, :], [wb, HTb[kc][t]], kc == 0, kc == 7)
            rb_ = RCb[blk // 4][blk % 4]
            S.op("act", lambda e, ps=ps, blk=blk: e.activation(out=rvall[:, blk, :], in_=ps[:, :], func=AF.Copy), reads=[pb], writes=[rb_])
            S.op("sp", lambda e, blk=blk: e.dma_start(out=lrv_v[:, :, blk, :], in_=rvall[:, blk, :].rearrange("p (h d) -> p h d", h=4)), reads=[rb_], writes=[pc_(f"rv{blk}")], dma=rb_)
        if stop == "m2d":
            return
        (w, wb), (w2, wb2) = wget(2)
        wv = w.rearrange("p (c k n) -> p c k n", c=4, k=8)
        wv2 = w2.rearrange("p (c k n) -> p c k n", c=4, k=8)

        def rope_pass(tab_c, tab_s, dst, dstb, post=None):
            kk = 0
            for t in range(NT):
                tb = t % 2
                S.op("sp", lambda e, t=t, tb=tb: e.dma_start(out=tabs[tb][0], in_=c_rope[tab_c][:, tsl(t)]), writes=[tabb[tb]], dma=tabb[tb])
                S.op("sp", lambda e, t=t, tb=tb: e.dma_start(out=tabs[tb][1], in_=c_rope[tab_s][:, tsl(t)]), writes=[tabb[tb]], dma=tabb[tb])
                for h in range(4):
                    p1, p1b = psbig()
                    p2, p2b = psbig()
                    proj_fm(wv, wb, h, t, p1[:, :], p1b)
                    proj_fm(wv2, wb2, h, t, p2[:, :], p2b)
                    i = kk % 2
                    kk += 1
                    S.op("dve", lambda e, p1=p1, i=i, tb=tb: e.tensor_tensor(out=t12[i][0], in0=p1[:, :], in1=tabs[tb][0], op=ALU.mult), reads=[p1b, tabb[tb]], writes=[t12b[i][0]])
                    S.op("dve", lambda e, p2=p2, i=i, tb=tb: e.tensor_tensor(out=t12[i][1], in0=p2[:, :], in1=tabs[tb][1], op=ALU.mult), reads=[p2b, tabb[tb]], writes=[t12b[i][1]])
                    if post is None:
                        S.op("pool", lambda e, i=i, h=h, t=t: e.tensor_tensor(out=dst[:, h, tsl(t)], in0=t12[i][0], in1=t12[i][1], op=ALU.add),
                             reads=[t12b[i][0], t12b[i][1]], writes=[dstb[h][t]])
                    else:
                        S.op("pool", lambda e, i=i: e.tensor_tensor(out=t12[i][0], in0=t12[i][0], in1=t12[i][1], op=ALU.add),
                             reads=[t12b[i][0], t12b[i][1]], writes=[t12b[i][0]])
                        post(i, h, t)
        rope_pass(2, 3, krT, krb)
        lkz_v = lkz.rearrange("p (h j d) -> p h j d", h=4, j=16)
        XF, bXF = sm["XF"], sm["bXF"]
        kk = 0
        for h in range(4):
            pfin, pfinb = ps5()
            for blk in range(NB):
                t = blk // 4
                pt, ptb = pst()
                S.op("pe", lambda e, pt=pt, h=h, blk=blk: e.transpose(pt, krT[:, h, blk * 128:(blk + 1) * 128], IDB), reads=[krb[h][t], bIDB], writes=[ptb])
                i = kk % 2
                kk += 1
                S.op("act", lambda e, pt=pt, i=i, h=h: e.activation(out=kst[i][0], in_=pt, func=AF.Copy, scale=c_zeta[:, h:h + 1]), reads=[ptb, bCF], writes=[kstb[i][0]])
                S.op("dve", lambda e, pt=pt, i=i, h=h, blk=blk: e.tensor_scalar(out=kst[i][1], in0=pt, scalar1=c_kdec[:, blk, h:h + 1], scalar2=None, op0=ALU.mult), reads=[ptb, bCF], writes=[kstb[i][1]])
                S.op("sp", lambda e, i=i, h=h, blk=blk: e.dma_start(out=lkz_v[:, h, blk, :], in_=kst[i][0]), reads=[kstb[i][0]], writes=[pc_(f"kz{h}_{blk}")], dma=kstb[i][0])
                mm(pfin, pfinb, kst[i][1], rvall[:, blk, h * 128:(h + 1) * 128], [kstb[i][1], RCb[blk // 4][blk % 4]], blk == 0, blk == NB - 1)
            S.op("act", lambda e, pfin=pfin, h=h: e.activation(out=XF[:, 264 + h * 128:264 + (h + 1) * 128], in_=pfin, func=AF.Copy), reads=[pfinb], writes=[bXF])
        if stop == "m2c":
            return
        S.op("pool", lambda e: e.dma_start(out=WFF, in_=ffw[l]), writes=[bWFF], dma=bWFF)
        wffv = WFF.rearrange("p (k n) -> p k n", k=8)
        pl, plb = psbig()
        for blk in range(NB):
            t = blk // 4
            for kc in range(8):
                mm(pl[:, blk * 8:(blk + 1) * 8], plb, HT[:, kc, blk * 128:(blk + 1) * 128], wffv[:, kc, :], [bWFF, HTb[kc][t]], kc == 0, kc == 7)
        LF, bLF = sm["LF"], sm["bLF"]
        LF3 = LF.rearrange("p (j h) -> p j h", j=16)
        bfb = c_bf[:, l * 8:(l + 1) * 8].unsqueeze(1).to_broadcast([128, 16, 8])
        S.op("dve", lambda e: e.tensor_tensor(out=LF3, in0=pl[:, 0:128].rearrange("p (j h) -> p j h", j=16), in1=bfb, op=ALU.add), reads=[plb, bCF], writes=[bLF])
        S.op("act", lambda e: e.activation(out=LF, in_=LF, func=AF.Exp, scale=-1.0), reads=[bLF], writes=[bLF])
        S.op("act", lambda e: e.activation(out=LF, in_=LF, func=AF.Ln, bias=1.0), reads=[bLF], writes=[bLF])
        S.op("dve", lambda e: e.tensor_scalar(out=LF, in0=LF, scalar1=-1.0, scalar2=None, op0=ALU.mult), reads=[bLF], writes=[bLF])
        RUN, bRUN = sm["RUN"], sm["bRUN"]
        S.op("dve", lambda e: e.memset(RUN[:, 0:8], 0.0), writes=[bRUN])
        for blk in range(NB):
            S.op("dve", lambda e, blk=blk: e.tensor_tensor(out=RUN[:, (blk + 1) * 8:(blk + 2) * 8], in0=RUN[:, blk * 8:(blk + 1) * 8], in1=LF[:, blk * 8:(blk + 1) * 8], op=ALU.add),
                 reads=[bRUN, bLF], writes=[bRUN])
        pc, pcb = psbig()
        for blk in range(NB):
            mm(pc[:, blk * 8:(blk + 1) * 8], pcb, c_tric, LF[:, blk * 8:(blk + 1) * 8], [bCF, bLF], True, False)
            mm(pc[:, blk * 8:(blk + 1) * 8], pcb, c_onesf, RUN[:, blk * 8:(blk + 1) * 8], [bCF, bRUN], False, True)
        mm(pc[:, 128:136], pcb, c_onesf, RUN[:, 128:136], [bCF, bRUN], True, True)
        CUM, bCUM = sm["CUM"], sm["bCUM"]
        S.op("act", lambda e: e.activation(out=CUM, in_=pc[:, 0:136], func=AF.Copy), reads=[pcb], writes=[bCUM])
        S.op("dve", lambda e: e.tensor_scalar(out=XF[:, 0:128], in0=CUM[:, 0:128], scalar1=-1.0, scalar2=None, op0=ALU.mult), reads=[bCUM], writes=[bXF])
        totb = CUM[:, 128:136].unsqueeze(1).to_broadcast([128, 16, 8])
        S.op("dve", lambda e: e.tensor_tensor(out=XF[:, 128:256].rearrange("p (j h) -> p j h", j=16), in0=totb, in1=CUM[:, 0:128].rearrange("p (j h) -> p j h", j=16), op=ALU.subtract),
             reads=[bCUM], writes=[bXF])
        S.op("dve", lambda e: e.tensor_copy(out=XF[:, 256:264], in_=sm["kmax"]), reads=[sm["bkmax"]], writes=[bXF])
        S.op("sp", lambda e: e.dma_start(out=exf[:, 0:1024], in_=XF), reads=[bXF], writes=[bEXF], dma=bXF)
        if stop == "m2e":
            return
        S.op("pool", lambda e: e.collective_compute("AllGather", ALU.bypass, replica_groups=rg_, ins=[EXF.ap().opt()], outs=[GXF.ap().opt()]),
             reads=[bEXF, bGXV], writes=[bGXF], dma=bGXF, cc=True)
        if stop == "m2f":
            return
        (w, wb), (w2, wb2) = wget(2)
        wv = w.rearrange("p (c k n) -> p c k n", c=4, k=8)
        wv2 = w2.rearrange("p (c k n) -> p c k n", c=4, k=8)

        def qpost(i, h, t):
            xib = c_xi[:, h, :].unsqueeze(1).to_broadcast([128, 4, 128])
            S.op("dve", lambda e: e.tensor_tensor(out=qxiT[:, h, tsl(t)].rearrange("p (j c) -> p j c", j=4), in0=t12[i][0].rearrange("p (j c) -> p j c", j=4), in1=xib, op=ALU.mult),
                 reads=[t12b[i][0], bCF], writes=[qxb[h][t]])
        rope_pass(0, 1, None, None, post=qpost)
        S.barrier(skip=WSLb)

        if stop == "m3":
            return
        (wg, wgb), = wget(1)
        wgv = wg.rearrange("p (c k n) -> p c k n", c=4, k=8)
        kz = carve(O_RD, 2048).rearrange("p (j d) -> p j d", j=16)
        rv = carve(O_RD + 2048, 2048).rearrange("p (j d) -> p j d", j=16)
        retT = [carve(O_RD + 4096 + i * 1024, 1024, F32) for i in range(2)]
        bkz, brv = Buf("kz"), Buf("rv")
        G0, G1, bG = sm["G0"], sm["G1"], sm["bG"]
        S.op("sp", lambda e: e.dma_start(out=G0, in_=gxf[0:128, 0:776]), reads=[bGXF], writes=[bG], dma=bG)
        S.op("sp", lambda e: e.dma_start(out=G1, in_=gxf[128:256, 0:264]), reads=[bGXF], writes=[bG], dma=bG)
        bret = [Buf("ret0"), Buf("ret1")]
        rsq = carve(O_RE, 512)
        rsd = carve(O_RE + 512, 1024, F32)
        rrs = carve(O_RE + 1536, 1024, F32)
        rsg = carve(O_RE + 2560, 1024, F32)
        brsq, brsd, brrs, brsg = Buf("rsq"), Buf("rsd"), Buf("rrs"), Buf("rsg")
        Spf, bSpf = sm["Spf"], sm["bSpf"]
        Pst, bPst = sm["Pst"], sm["bPst"]
        Suse, bSuse = sm["Suse"], sm["bSuse"]
        PTr, bPTr = sm["PTr"], sm["bPTr"]
        YrT, yrb = RC, RCb
        for h in range(4):
            g_c = GAMMA[h] ** 128
            S.op("sp", lambda e, h=h: e.dma_start(out=kz, in_=lkz_v[:, h, :, :]), reads=[pc_(f"kz{h}_{b_}") for b_ in range(NB)], writes=[bkz], dma=bkz)
            S.op("sp", lambda e, h=h: e.dma_start(out=rv, in_=lrv_v[:, h, :, :]), reads=[pc_(f"rv{b_}") for b_ in range(NB)], writes=[brv], dma=brv)
            S.op("dve", lambda e, h=h: e.tensor_scalar(out=Spf, in0=G0[:, 264 + h * 128:264 + (h + 1) * 128], scalar1=c_fl[:, 0:1], scalar2=None, op0=ALU.mult), reads=[bG, bCF], writes=[bSpf])
            S.op("pool", lambda e: e.memset(Pst, 0.0), writes=[bPst])
            for j in range(NB):
                t = j // 4
                jj = j % 4
                csl = slice(j * 128, (j + 1) * 128)
                p1, p1b = ps4()
                mm(p1, p1b, krT[:, h, csl], qxiT[:, h, csl], [krb[h][t], qxb[h][t]], True, True)
                i = j % 2
                S.op("dve", lambda e, p1=p1, i=i, h=h: e.tensor_tensor(out=PTr[i], in0=p1, in1=c_decm[:, h, :], op=ALU.mult), reads=[p1b, bCF], writes=[bPTr[i]])
                S.op("dve", lambda e, i=i, j=j, g_c=g_c: e.scalar_tensor_tensor(out=Suse[i], in0=Spf, scalar=float(g_c ** j), in1=Pst, op0=ALU.mult, op1=ALU.add),
                     reads=[bSpf, bPst], writes=[bSuse[i]])
                po, pob = ps5()
                mm(po, pob, rv[:, j, :], PTr[i], [brv, bPTr[i]], True, False)
                mm(po, pob, Suse[i], qxiT[:, h, csl], [bSuse[i], qxb[h][t]], False, True)
                rt, rtb = retT[t % 2], bret[t % 2]
                S.op("act", lambda e, po=po, rt=rt, jj=jj: e.activation(out=rt[:, jj * 128:(jj + 1) * 128], in_=po, func=AF.Copy), reads=[pob], writes=[rtb])
                pk, pkb = ps4()
                mm(pk, pkb, kz[:, j, :], rv[:, j, :], [bkz, brv], True, True)
                S.op("dve", lambda e, pk=pk, g_c=g_c: e.scalar_tensor_tensor(out=Pst, in0=Pst, scalar=float(g_c), in1=pk, op0=ALU.mult, op1=ALU.add),
                     reads=[pkb, bPst], writes=[bPst])
                if jj == 3:
                    S.op("act", lambda e, rt=rt: e.activation(out=rsq, in_=rt, func=AF.Square), reads=[rtb], writes=[brsq])
                    pn, pnb = psbig()
                    mm(pn[:, :], pnb, ONB, rsq, [bONB, brsq], True, True)
                    S.op("act", lambda e, pn=pn: e.activation(out=rsd, in_=pn[:, :], func=AF.Sqrt, scale=1.0 / 128, bias=EPS), reads=[pnb], writes=[brsd])
                    S.op("dve", lambda e: e.reciprocal(out=rrs, in_=rsd), reads=[brsd], writes=[brrs])
                    pg, pgb = psbig()
                    proj_fm(wgv, wgb, h, t, pg[:, :], pgb)
                    S.op("act", lambda e, pg=pg: e.activation(out=rsg, in_=pg[:, :], func=AF.Silu), reads=[pgb], writes=[brsg])
                    S.op("dve", lambda e, rt=rt, h=h: e.scalar_tensor_tensor(out=rsd, in0=rt, scalar=c_rnw[:, l * 4 + h:l * 4 + h + 1], in1=rrs, op0=ALU.mult, op1=ALU.mult),
                         reads=[rtb, brrs, bCF, brsd], writes=[brsd])
                    S.op("dve", lambda e, h=h, t=t: e.tensor_tensor(out=YrT[:, h, tsl(t)], in0=rsd, in1=rsg, op=ALU.mult), reads=[brsd, brsg], writes=[yrb[h][t]])
        S.barrier(skip=WSLb)

        if stop == "m4":
            return
        (wq, wqb), = wget(1)
        wqv = wq.rearrange("p (c k n) -> p c k n", c=4, k=8)
        KT = carve(O_RD, 4096)
        VA = [carve(O_RD + 4096 + i * 2112, 2112).rearrange("p (j e) -> p j e", j=32) for i in range(2)]
        fqT = carve(O_RE, 2048)
        qsq2 = carve(O_RE + 2048, 512)
        bKT, bVA, bfq, bqs2 = Buf("KT"), [Buf("va0"), Buf("va1")], [Buf(f"fq{t}") for t in range(NT)], Buf("qs2")
        for i in range(2):
            S.op("pool", lambda e, i=i: e.memset(VA[i][:, :, 64:66], 1.0), writes=[bVA[i]])
        NA, NAG, NBt, bNT = sm["NA"], sm["NAG"], sm["NBt"], sm["bNT"]
        S.op("dve", lambda e: e.tensor_scalar(out=NA, in0=G0[:, 0:128], scalar1=c_fl[:, 1:2], scalar2=None, op0=ALU.mult), reads=[bG, bCF], writes=[bNT])
        S.op("dve", lambda e: e.scalar_tensor_tensor(out=NA, in0=G0[:, 128:256], scalar=c_fl[:, 0:1], in1=NA, op0=ALU.mult, op1=ALU.add), reads=[bG, bCF, bNT], writes=[bNT])
        S.op("dve", lambda e: e.tensor_scalar(out=NAG, in0=G0[:, 128:256], scalar1=c_fl[:, 0:1], scalar2=c_fl[:, 2:3], op0=ALU.mult, op1=ALU.add), reads=[bG, bCF], writes=[bNT])
        S.op("dve", lambda e: e.tensor_scalar(out=NBt, in0=G1[:, 0:128], scalar1=c_fl[:, 0:1], scalar2=c_fl[:, 2:3], op0=ALU.mult, op1=ALU.add), reads=[bG, bCF], writes=[bNT])
        KM, bKM = sm["KM"], sm["bKM"]
        S.op("dve", lambda e: e.tensor_tensor(out=KM, in0=G0[:, 256:264], in1=G1[:, 256:264], op=ALU.max), reads=[bG], writes=[bKM])
        NA3 = NA.rearrange("p (j h) -> p j h", j=16)
        NAG3 = NAG.rearrange("p (j h) -> p j h", j=16)
        NB3 = NBt.rearrange("p (j h) -> p j h", j=16)
        CUM3 = CUM[:, 0:128].rearrange("p (j h) -> p j h", j=16)
        gxb_fk = [gxk[r * 128:(r + 1) * 128, :].rearrange("p (c t) -> p c t", c=4) for r in range(2)]
        gxb_fv = [gxv[r * 128:(r + 1) * 128, :].rearrange("p (h j e) -> p h j e", h=8, j=16) for r in range(2)]
        foxT, foxb = RB, RBb
        QM, bQM = sm["QM"], sm["bQM"]
        MC, bMC = sm["MC"], sm["bMC"]
        CM, bCM = sm["CM"], sm["bCM"]
        DG, bDG = sm["DG"], sm["bDG"]
        BQ, bBQ = sm["BQ"], sm["bBQ"]
        TT, bTT = sm["TT"], sm["bTT"]
        PT, bPT = sm["PT"], sm["bPT"]
        FT, bFT = sm["FT"], sm["bFT"]
        REC, bREC = sm["REC"], sm["bREC"]
        TT2 = [carve(O_RE + 2560 + i * 512, 512, F32) for i in range(3)]
        BQ4 = {(0, 0): (BQ[0], bBQ[0]), (0, 1): (BQ[1], bBQ[1]), (1, 0): (G1[:, 0:128], bG), (1, 1): (G1[:, 128:256], bG)}
        PT2 = [sm["TT"][i].bitcast(BF16) for i in range(3)]
        bTT2 = [Buf(f"tt2{i}") for i in range(3)]
        bPT2 = [Buf(f"pt2{i}") for i in range(3)]
        psA = [(PSB[0], psbig_b[0]), (PSB[1], psbig_b[1]), (PSB[3], ps4.bufs[0]), (PSB[4], ps4.bufs[1])]
        psa_i = [0]
        poA = [(PSB[5], ps5.bufs[0]), (PSB[6], ps6.bufs[0])]
        for hp in range(4):
            for r in range(2):
                S.op("sp", lambda e, hp=hp, r=r: e.dma_start(out=KT[:, r * T:(r + 1) * T], in_=gxb_fk[r][:, hp, :]), reads=[bGXK], writes=[bKT], dma=bKT)
                for hh in range(2):
                    S.op("sp", lambda e, hp=hp, r=r, hh=hh: e.dma_start(out=VA[hh][:, r * 16:(r + 1) * 16, 0:64], in_=gxb_fv[r][:, hp * 2 + hh, :, :]), reads=[bGXV], writes=[bVA[hh]], dma=bVA[hh])
            S.op("pool", lambda e: e.memset(QM, 0.0), writes=[bQM])
            for t in range(NT):
                ps, pb = psbig()
                proj_fm(wqv, wqb, hp, t, ps[:, :], pb)
                S.op("act", lambda e, ps=ps, t=t: e.activation(out=fqT[:, tsl(t)], in_=ps[:, :], func=AF.Copy, scale=scale), reads=[pb], writes=[bfq[t]])
                S.op("act", lambda e, ps=ps: e.activation(out=qsq2, in_=ps[:, :], func=AF.Square), reads=[pb], writes=[bqs2])
                for hh in range(2):
                    p2, p2b = psbig()
                    mm(p2[:, :], p2b, SEL2B[:, hh, :], qsq2, [bSEL, bqs2], True, True)
                    S.op("dve", lambda e, p2=p2: e.reduce_max(out=sm["red"], in_=p2[:, :], axis=AX.X), reads=[p2b], writes=[sm["bred"]])
                    S.op("dve", lambda e, hh=hh: e.tensor_tensor(out=QM[:, hh:hh + 1], in0=QM[:, hh:hh + 1], in1=sm["red"], op=ALU.max), reads=[sm["bred"], bQM], writes=[bQM])
            S.op("dve", lambda e, hp=hp: e.tensor_tensor(out=MC, in0=QM, in1=KM[:, hp * 2:hp * 2 + 2], op=ALU.mult), reads=[bQM, bKM], writes=[bMC])
            S.op("act", lambda e: e.activation(out=MC, in_=MC, func=AF.Sqrt, scale=(1.05 * scale) ** 2), reads=[bMC], writes=[bMC])
            for hh in range(2):
                h = hp * 2 + hh
                S.op("dve", lambda e, h=h, hh=hh: e.tensor_scalar(out=CM[:, hh * 16:(hh + 1) * 16], in0=CUM3[:, :, h], scalar1=MC[:, hh:hh + 1], scalar2=None, op0=ALU.subtract),
                     reads=[bCUM, bMC], writes=[bCM])
            units = []
            for j in range(NB):
                steps = [(0, g) for g in range(16)] + [(1, g) for g in range(j + 1)]
                for si, (half, g) in enumerate(steps):
                    units.append({"j": j, "si": si, "half": half, "g": g, "n": len(steps)})
            LA = 2

            def emit_S(u, hp=hp):
                j = u["j"]
                qsl = slice(j * 128, (j + 1) * 128)
                if u["si"] == 0:
                    for hh in range(2):
                        S.op("dve", lambda e, hh=hh: e.tensor_scalar(out=DG, in0=c_identf, scalar1=CM[:, hh * 16 + j:hh * 16 + j + 1], scalar2=None, op0=ALU.mult), reads=[bCF, bCM], writes=[bDG])
                        pbq, pbqb = PSB[2], psbig_b[2]
                        mm(pbq[:, 0:128], pbqb, c_onesf, DG, [bCF, bDG], True, True)
                        bq_, bqb_ = BQ4[(j % 2, hh)]
                        S.op("dve", lambda e, bq_=bq_: e.tensor_copy(out=bq_, in_=pbq[:, 0:128]), reads=[pbqb], writes=[bqb_])
                u["p1"] = []
                ksl = slice(u["half"] * T + u["g"] * 128, u["half"] * T + (u["g"] + 1) * 128)
                for hh in range(2):
                    rows = slice(hh * 64, (hh + 1) * 64)
                    k_ = psa_i[0] % 4
                    psa_i[0] += 1
                    p1, p1b = psA[k_]
                    mm(p1[:, 0:128], p1b, KT[rows, ksl], fqT[rows, qsl], [bKT, bfq[j // 4]], True, True)
                    u["p1"].append((p1, p1b))

            def emit_rest(u, ui, hp=hp):
                j, si, half, g = u["j"], u["si"], u["half"], u["g"]
                i = ui % 3
                if half == 0:
                    tab = NA3 if g <= j else NAG3
                else:
                    tab = NB3
                for hh in range(2):
                    h = hp * 2 + hh
                    p1, p1b = u["p1"][hh]
                    bq_, bqb_ = BQ4[(j % 2, hh)]
                    S.op("dve", lambda e, hh=hh, h=h, p1=p1, bq_=bq_: e.scalar_tensor_tensor(out=TT2[i][:, hh * 128:(hh + 1) * 128], in0=p1[:, 0:128], scalar=tab[:, g, h:h + 1], in1=bq_, op0=ALU.add, op1=ALU.add),
                         reads=[p1b, bqb_, bNT], writes=[bTT2[i]])
                S.op("act", lambda e: e.activation(out=PT2[i], in_=TT2[i], func=AF.Exp), reads=[bTT2[i]], writes=[bPT2[i]])
                if g == j:
                    msk, mb = (MAB, bMAB) if half == 0 else (TRIB, bTRIB)
                    S.op("pool", lambda e: e.tensor_tensor(out=PT2[i].rearrange("p (a q) -> p a q", a=2), in0=PT2[i].rearrange("p (a q) -> p a q", a=2), in1=msk.unsqueeze(1).to_broadcast([128, 2, 128]), op=ALU.mult),
                         reads=[bPT2[i], mb], writes=[bPT2[i]])
                for hh in range(2):
                    po, pob = poA[hh]
                    mm(po[:, 0:66], pob, PT2[i][:, hh * 128:(hh + 1) * 128], VA[hh][:, half * 16 + g, :], [bPT2[i], bVA[hh]], si == 0, si == u["n"] - 1)
                if si == u["n"] - 1:
                    fi = j % 2
                    for hh in range(2):
                        po, pob = poA[hh]
                        S.op("dve", lambda e, po=po: e.reciprocal(out=REC, in_=po[:, 64:65]), reads=[pob], writes=[bREC])
                        S.op("act", lambda e, po=po, hh=hh: e.activation(out=FT[fi][:, hh * 64:(hh + 1) * 64], in_=po[:, 0:64], func=AF.Copy, scale=REC), reads=[pob, bREC], writes=[bFT[fi]])
                    qsl = slice(j * 128, (j + 1) * 128)
                    pt, ptb = pst()
                    S.op("pe", lambda e: e.transpose(pt, FT[fi], IDB), reads=[bFT[fi], bIDB], writes=[ptb])
                    S.op("dve", lambda e: e.tensor_copy(out=foxT[:, hp, qsl], in_=pt), reads=[ptb], writes=[foxb[hp][j // 4]])

            for idx in range(len(units) + LA):
                if idx - LA >= 0:
                    emit_rest(units[idx - LA], idx - LA)
                if idx < len(units):
                    emit_S(units[idx])
        S.barrier(skip=WSLb)

        if stop == "m5":
            return
        mg = carve(O_RD, 8192).rearrange("p (c t) -> p c t", c=4)
        mgb = [[Buf(f"mg{c}_{t}") for t in range(NT)] for c in range(4)]
        tm = [carve(O_RE + i * 1024, 1024, F32) for i in range(4)]
        tmb = [Buf(f"tm{i}") for i in range(4)]
        halves = []
        for oh in range(2):
            (wo, wob), (wr, wrb), (wf, wfb) = wget(3)
            halves.append(None)
            wov = wo.rearrange("p (a k n) -> p a k n", a=2, k=4)
            wrv = wr.rearrange("p (c k n) -> p c k n", c=4, k=8)
            wfv = wf.rearrange("p (c k n) -> p c k n", c=4, k=8)
            for oc in range(4):
                for t in range(NT):
                    pyr, pyrb = ps7()
                    for kc in range(4):
                        mm(pyr[:, :], pyrb, wov[:, 0, kc, oc * 128:(oc + 1) * 128], YrT[:, kc, tsl(t)], [wob, yrb[kc][t]], kc == 0, kc == 3)
                    pgr, pgrb = ps7()
                    proj_fm(wrv, wrb, oc, t, pgr[:, :], pgrb)
                    S.op("act", lambda e, pgr=pgr: e.activation(out=tm[0], in_=pgr[:, :], func=AF.Sigmoid), reads=[pgrb], writes=[tmb[0]])
                    S.op("dve", lambda e, pyr=pyr: e.tensor_tensor(out=tm[1], in0=pyr[:, :], in1=tm[0], op=ALU.mult), reads=[pyrb, tmb[0]], writes=[tmb[1]])
                    pyf, pyfb = ps7()
                    for kc in range(4):
                        mm(pyf[:, :], pyfb, wov[:, 1, kc, oc * 128:(oc + 1) * 128], foxT[:, kc, tsl(t)], [wob, foxb[kc][t]], kc == 0, kc == 3)
                    pgf, pgfb = ps7()
                    proj_fm(wfv, wfb, oc, t, pgf[:, :], pgfb)
                    S.op("act", lambda e, pgf=pgf: e.activation(out=tm[2], in_=pgf[:, :], func=AF.Sigmoid), reads=[pgfb], writes=[tmb[2]])
                    S.op("dve", lambda e, pyf=pyf: e.tensor_tensor(out=tm[3], in0=pyf[:, :], in1=tm[2], op=ALU.mult), reads=[pyfb, tmb[2]], writes=[tmb[3]])
                    S.op("pool", lambda e, oc=oc, t=t: e.tensor_tensor(out=mg[:, oc, tsl(t)], in0=tm[1], in1=tm[3], op=ALU.add), reads=[tmb[1], tmb[3]], writes=[mgb[oc][t]])
            (ww, wwb), = wget(1)
            wwv = ww.rearrange("p (k n) -> p k n", k=4)
            for o in range(8):
                for t in range(NT):
                    ps, pb = ps7()
                    for kc in range(4):
                        mm(ps[:, :], pb, wwv[:, kc, o * 128:(o + 1) * 128], mg[:, kc, tsl(t)], [wwb, mgb[kc][t]], kc == 0, kc == 3)
                    S.op("dve", lambda e, ps=ps, o=o, t=t: e.tensor_tensor(out=XT[:, o, tsl(t)], in0=ps[:, :], in1=XT[:, o, tsl(t)], op=ALU.add),
                         reads=[pb, XTb[o][t]], writes=[XTb[o][t]])

    sm = {}

    def smt(name, ncols, dt=F32, n=1):
        if n == 1:
            sm[name] = rs(ncols * (2 if dt == F32 else 1), dt)
            sm["b" + name] = Buf(name)
        else:
            sm[name] = [rs(ncols * (2 if dt == F32 else 1), dt) for _ in range(n)]
            sm["b" + name] = [Buf(f"{name}{i}") for i in range(n)]
    smt("kmax", 8); smt("red", 1); smt("LF", 128); smt("RUN", 136); smt("CUM", 136); smt("XF", 1024)
    smt("Spf", 128); smt("Pst", 128); smt("Suse", 128, BF16, 2); smt("PTr", 128, BF16, 2)
    smt("G0", 776); smt("G1", 264)
    sm["NA"] = rs(256, F32); sm["NAG"] = rs(256, F32); sm["NBt"] = rs(256, F32); sm["bNT"] = Buf("nt")
    sm["bG"] = Buf("g01")
    smt("KM", 8); smt("QM", 2); smt("MC", 2); smt("CM", 32); smt("DG", 128)
    smt("BQ", 128, F32, 2); smt("TT", 128, F32, 3); smt("PT", 128, BF16, 3); smt("FT", 128, BF16, 2); smt("REC", 1)
    mixer.small = sm
    S.op("pool", lambda e: e.memset(sm["XF"], 0.0), writes=[sm["bXF"]])

    for l in range(nl):
        ffn(l, 0)
        if stop == "ffn1":
            break
        mixer(l)
        if stop is not None:
            break
        ffn(l, 1)
    S.barrier(skip=WSLb)
    rmsnorm(12 * 8, final=True)
    ov = outT.rearrange("(c p) t -> p c t", p=128)
    bo = Buf("ostore")
    for c in range(8):
        S.op("sp", lambda e, c=c: e.dma_start(out=ov[:, c, :], in_=XT[:, c, :]), reads=XTb[c], dma=bo)
    S.barrier()
    print('nsem', S.nsem, 'nops', S.nops, {e: len(S.lists[e]) for e in S.ENGS})
    S.emit()
    return nc


def _fm(Wc):
    return np.ascontiguousarray(Wc.reshape(8, 128, 4, 128).transpose(1, 2, 0, 3)).reshape(128, 4096)


def _tm(Wc):
    return np.ascontiguousarray(Wc.reshape(8, 128, 512).transpose(1, 0, 2)).reshape(128, 4096)


def _swap_cols(Wc):
    w = Wc.reshape(Wc.shape[0], 4, 2, 64)
    return w[:, :, ::-1, :].reshape(Wc.shape[0], 512)


def _ffn_tiles(w_in, w_out):
    tiles = []
    for gi in range(6):
        nch = 4 if gi < 5 else 2
        for ip in range(nch // 2):
            p = gi * 2 + ip
            cols = np.concatenate([w_in[:, p * 256:(p + 1) * 256], w_in[:, DFF + p * 256:DFF + (p + 1) * 256]], axis=1)
            tiles.append(_fm(cols))
        wo = np.zeros((4, 128, 1024), np.float32)
        wo[:nch] = w_out[gi * 512: gi * 512 + nch * 128].reshape(nch, 128, 1024)
        tiles.append(np.ascontiguousarray(wo.transpose(1, 0, 2)).reshape(128, 4096))
    return tiles


def _mixer_tiles(w_in, w_o_ret, w_o_fox, w_out):
    t = []
    t.append(_fm(w_in[:, 2560:3072]))
    t.append(_tm(w_in[:, 3072:3584]))
    t.append(_tm(w_in[:, 1024:1536]))
    t.append(_fm(w_in[:, 512:1024]))
    t.append(_fm(_swap_cols(w_in[:, 512:1024])))
    t.append(_fm(w_in[:, 0:512]))
    t.append(_fm(_swap_cols(w_in[:, 0:512])))
    t.append(_fm(w_in[:, 1536:2048]))
    t.append(_fm(w_in[:, 2048:2560]))
    for oh in range(2):
        a = np.stack([w_o_ret[:, oh * 512:(oh + 1) * 512].reshape(4, 128, 512), w_o_fox[:, oh * 512:(oh + 1) * 512].reshape(4, 128, 512)], 0)
        t.append(np.ascontiguousarray(a.transpose(2, 0, 1, 3)).reshape(128, 4096))
        t.append(_fm(w_in[:, 3592 + oh * 512:3592 + (oh + 1) * 512]))
        t.append(_fm(w_in[:, 4616 + oh * 512:4616 + (oh + 1) * 512]))
        wo = w_out[oh * 512:(oh + 1) * 512].reshape(4, 128, 1024)
        t.append(np.ascontiguousarray(wo.transpose(1, 0, 2)).reshape(128, 4096))
    return t


def _consts(rank, norm_ffn1, norm_mix, norm_ffn2, norm_final, ret_norm, b_forget):
    cf = np.zeros((128, 2176), np.float32)
    cf[:, 0:128] = np.eye(128, dtype=np.float32)
    cf[:, 128:256] = 1.0
    idx = np.arange(128)
    cf[:, 256:384] = (idx[:, None] <= idx[None, :]).astype(np.float32)
    g = np.array(GAMMA, np.float64)
    lg = np.log(g)
    decm = np.where(idx[None, None, :] >= idx[:, None, None], np.exp(-lg[None, :, None] * (idx[:, None, None] + 1.0)), 0.0)
    cf[:, 384:896] = decm.reshape(128, 512)
    xi = np.exp(lg[:, None] * (idx[None, :] + 1.0))
    cf[:, 896:1408] = np.broadcast_to(xi.reshape(1, 512), (128, 512))
    cf[:, 1408:1412] = np.exp(lg[None, :] * (127.0 - idx[:, None]))
    tt = (np.arange(16)[None, :, None] * 128 + idx[:, None, None]).astype(np.float64)
    cf[:, 1412:1476] = np.exp(lg[None, None, :] * (2047.0 - tt)).reshape(128, 64)
    fl = float(rank)
    cf[:, 1476] = fl
    cf[:, 1477] = 1.0 - fl
    cf[:, 1478] = (1.0 - fl) * -30000.0
    gn = np.zeros((13, 1024), np.float32)
    for l in range(DEPTH):
        gn[l * 3 + 0] = norm_ffn1[l]
        gn[l * 3 + 1] = norm_mix[l]
        gn[l * 3 + 2] = norm_ffn2[l]
    gn[12] = norm_final
    cf[:, 1480:1584] = gn.reshape(13, 8, 128).transpose(2, 0, 1).reshape(128, 104)
    cf[:, 1584:1600] = ret_norm.reshape(4, 4, 128).transpose(2, 0, 1).reshape(128, 16)
    cf[:, 1600:1632] = np.broadcast_to(b_forget.reshape(1, 32), (128, 32))
    tri = (idx[:, None] <= idx[None, :]).astype(np.float32)
    cf[:, 1632:1760] = tri
    cf[:, 1760:1888] = tri if rank == 0 else 1.0
    sel = np.zeros((128, 2, 128), np.float32)
    sel[0:64, 0, :] = 1.0
    sel[64:128, 1, :] = 1.0
    cf[:, 1888:2144] = sel.reshape(128, 256)
    pos = (rank * T + np.arange(T)).astype(np.float32)
    inv = np.power(np.float32(10000.0), -np.arange(0, 128, 2, dtype=np.float32) / np.float32(128)).astype(np.float32)
    ang = (pos[:, None] * inv[None, :]).astype(np.float32)
    cos = np.cos(ang).T.astype(np.float32)
    sin = np.sin(ang).T.astype(np.float32)
    COS = np.concatenate([cos, cos], 0)
    SIN = np.concatenate([-sin, sin], 0)
    ks = np.float32(128 ** -0.5)
    rope = np.stack([COS, SIN, COS * ks, SIN * ks], 0).astype(np.float32)
    return cf, rope


_CACHE = {}


def kernel(x, norm_ffn1, w_ffn1_in, w_ffn1_out, norm_mix, w_in, b_forget, ret_norm,
           w_o_ret, w_o_fox, w_out, norm_ffn2, w_ffn2_in, w_ffn2_out, norm_final, _nl=DEPTH, _stop=None):
    f = lambda a: np.asarray(a, dtype=np.float32)
    x = f(x)
    nl = _nl
    tiles = []
    for l in range(nl):
        tiles += _ffn_tiles(f(w_ffn1_in[l]), f(w_ffn1_out[l]))
        tiles += _mixer_tiles(f(w_in[l]), f(w_o_ret[l]), f(w_o_fox[l]), f(w_out[l]))
        tiles += _ffn_tiles(f(w_ffn2_in[l]), f(w_ffn2_out[l]))
    wseq = np.stack(tiles, 0)
    assert wseq.shape[0] == nl * TPL, wseq.shape
    ffw = np.stack([np.ascontiguousarray(f(w_in[l])[:, 3584:3592].reshape(8, 128, 8).transpose(1, 0, 2)).reshape(128, 64) for l in range(nl)], 0)
    in_maps = []
    for c in range(8):
        b, r = c // 2, c % 2
        cf, rope = _consts(r, f(norm_ffn1), f(norm_mix), f(norm_ffn2), f(norm_final), f(ret_norm), f(b_forget))
        xT = np.ascontiguousarray(x[b, r * T:(r + 1) * T, :].T)
        in_maps.append({"xT": xT, "wseq": wseq, "ffw": ffw, "c_f32": cf, "c_rope": rope})
    if (nl, _stop) not in _CACHE:
        _CACHE[(nl, _stop)] = build_nc(nl, _stop)
    nc = _CACHE[(nl, _stop)]
    res = run_bass_kernel_spmd(nc, in_maps, core_ids=list(range(8)))
    out = np.empty((NBATCH, SEQ, D), np.float32)
    for c in range(8):
        b, r = c // 2, c % 2
        out[b, r * T:(r + 1) * T, :] = res.results[c]["outT"].T
    return out
```

```python
import numpy as np
from contextlib import ExitStack
import concourse.bass as bass
import concourse.mybir as mybir
from concourse.bass_utils import run_bass_kernel_spmd

F32 = mybir.dt.float32
BF16 = mybir.dt.bfloat16
AF = mybir.ActivationFunctionType
ALU = mybir.AluOpType
AX = mybir.AxisListType

D = 1024
NBATCH = 4
SEQ = 4096
DEPTH = 4
T = 2048
NT = 4
NB = 16
DFF = 2816
EPS = 1e-6
TPL = 51
NCF = 1024
GAMMA = [1.0 - 2.0 ** (-5 - h) for h in range(4)]


class Buf:
    __slots__ = ("name", "lw", "rd", "dsem", "dcnt", "dlast")

    def __init__(self, name):
        self.name = name
        self.lw = None
        self.rd = {}
        self.dsem = None
        self.dcnt = 0
        self.dlast = None


class Sched:
    ENGS = ("pe", "act", "dve", "pool", "sp")
    EPOCH = 30000

    def __init__(self, nc):
        self.nc = nc
        self.lists = {e: [] for e in self.ENGS}
        self.cnt = {e: 0 for e in self.ENGS}
        self.nsem = 0
        self.sem = {e: self._new_sem(e) for e in self.ENGS}
        self.waited = {e: {} for e in self.ENGS}
        self.last = {e: None for e in self.ENGS}
        self.dmabufs = []
        self.nops = 0

    def _new_sem(self, tag):
        self.nsem += 1
        return self.nc.alloc_semaphore(name=f"s{self.nsem}_{tag}")

    def _wait(self, eng, waits, t):
        sem, val = t[0], t[1]
        wd = self.waited[eng]
        k = id(sem)
        if wd.get(k, 0) >= val:
            return
        wd[k] = val
        waits.append((sem, val))

    def op(self, eng, fn, reads=(), writes=(), dma=None, cc=False):
        waits = []
        deps = []
        for b in reads:
            if b.lw is not None:
                deps.append((b.lw, 0))
        for b in writes:
            if b.lw is not None:
                deps.append((b.lw, 0))
            for t in b.rd.values():
                deps.append((t, 1))
        if dma is not None and dma.dlast is not None:
            deps.append((dma.dlast, 0))
        for (t, war) in deps:
            if (not t[3]) and t[2] == eng and eng == "pe":
                continue
            self._wait(eng, waits, t)
        if dma is not None:
            if dma.dsem is None or dma.dcnt >= 48000 or cc:
                dma.dsem = self._new_sem("d")
                dma.dcnt = 0
                if dma not in self.dmabufs:
                    self.dmabufs.append(dma)
            inc = 1 if cc else 16
            dma.dcnt += inc
            ticket = (dma.dsem, dma.dcnt, eng, True)
            dma.dlast = ticket
        else:
            if self.cnt[eng] >= self.EPOCH:
                self.sem[eng] = self._new_sem(eng)
                self.cnt[eng] = 0
            self.cnt[eng] += 1
            ticket = (self.sem[eng], self.cnt[eng], eng, False)
            self.last[eng] = ticket
            inc = 1
        self.lists[eng].append((waits, fn, ticket[0], inc, cc))
        self.nops += 1
        for b in writes:
            b.lw = ticket
            b.rd = {}
        for b in reads:
            if b.lw is not ticket:
                b.rd[id(ticket[0])] = ticket
        return ticket

    def barrier(self, skip=()):
        ts = [self.last[e] for e in self.ENGS if self.last[e] is not None]
        for b in self.dmabufs:
            if b.dlast is not None and b not in skip:
                ts.append(b.dlast)
        for e in self.ENGS:
            waits = []
            for t in ts:
                if (not t[3]) and t[2] == e:
                    continue
                self._wait(e, waits, t)
            if waits:
                self.lists[e].append((waits, None, None, 0, False))

    def emit(self):
        nc = self.nc
        with nc.Block() as block:
            def mk(ename):
                lst = self.lists[ename]

                def body(e):
                    for (waits, fn, sem, inc, cc) in lst:
                        for (s, v) in waits:
                            e.wait_ge(s, v)
                        if fn is not None:
                            ins = fn(e)
                            if cc:
                                ins.then_inc(sem)
                            else:
                                ins.then_inc(sem, inc)
                return body
            block.tensor(mk("pe"))
            block.scalar(mk("act"))
            block.vector(mk("dve"))
            block.gpsimd(mk("pool"))
            block.sync(mk("sp"))


def build_nc(nl=DEPTH, stop=None):
    nc = bass.Bass("TRN2", target_bir_lowering=False)
    es = ExitStack()
    S = Sched(nc)

    def din(name, shape, dt=F32):
        return nc.dram_tensor(name, list(shape), dt, kind="ExternalInput").ap()

    xT_in = din("xT", [D, T])
    wseq = din("wseq", [nl * TPL, 128, 4096])
    ffw = din("ffw", [nl, 128, 64])
    c_f32 = din("c_f32", [128, 2176])
    c_rope = din("c_rope", [4, 128, T])
    outT = nc.dram_tensor("outT", [D, T], F32, kind="ExternalOutput").ap()
    EXK = nc.dram_tensor("exk", [128, 8192], BF16)
    GXK = nc.dram_tensor("gxk", [256, 8192], BF16)
    EXV = nc.dram_tensor("exv", [128, 8192], BF16)
    GXV = nc.dram_tensor("gxv", [256, 8192], BF16)
    EXF = nc.dram_tensor("exf", [128, NCF], F32)
    GXF = nc.dram_tensor("gxf", [256, NCF], F32)
    LKZ = nc.dram_tensor("lkz", [128, 8192], BF16)
    LRV = nc.dram_tensor("lrv", [128, 8192], BF16)
    exk, gxk, exv, gxv, exf, gxf, lkz, lrv = EXK.ap(), GXK.ap(), EXV.ap(), GXV.ap(), EXF.ap(), GXF.ap(), LKZ.ap(), LRV.ap()
    bGXK, bGXV, bEXF, bGXF = Buf("gxk"), Buf("gxv"), Buf("exf"), Buf("gxf")
    pieces = {}

    def pc_(name):
        if name not in pieces:
            pieces[name] = Buf(name)
        return pieces[name]

    NCOL = 104448
    REG = es.enter_context(nc.sbuf_tensor("reg", [128, NCOL], BF16))
    PSB = [es.enter_context(nc.psum_tensor(f"psb{i}", [128, 512], F32)) for i in range(7)]
    PST = es.enter_context(nc.psum_tensor("pst", [128, 1024], BF16))

    def carve(off, ncols, dt=BF16, **rr):
        ap = REG[:, off:off + ncols]
        if dt == F32:
            ap = ap.bitcast(F32)
        return ap

    O_XT, O_HT, O_WR, O_RB, O_RC, O_RD, O_RE, O_RS = 0, 32768, 49152, 61440, 69632, 77824, 86144, 90240
    XT = carve(O_XT, 32768, F32).rearrange("p (c t) -> p c t", c=8)
    HT = carve(O_HT, 16384).rearrange("p (c t) -> p c t", c=8)
    XTb = [[Buf(f"xt{c}_{t}") for t in range(NT)] for c in range(8)]
    HTb = [[Buf(f"ht{c}_{t}") for t in range(NT)] for c in range(8)]
    WSL = [carve(O_WR + i * 4096, 4096) for i in range(3)]
    WSLb = [Buf(f"w{i}") for i in range(3)]
    RB = carve(O_RB, 8192).rearrange("p (c t) -> p c t", c=4)
    RBb = [[Buf(f"rb{c}_{t}") for t in range(NT)] for c in range(4)]
    RC = carve(O_RC, 8192).rearrange("p (c t) -> p c t", c=4)
    RCb = [[Buf(f"rc{c}_{t}") for t in range(NT)] for c in range(4)]

    rs_off = [O_RS]

    def rs(ncols, dt=BF16):
        o = rs_off[0]
        rs_off[0] += ncols
        assert rs_off[0] <= NCOL, rs_off[0]
        return carve(o, ncols, dt)

    CF = rs(4352, F32)
    bCF = Buf("cf")
    c_identf = CF[:, 0:128]
    c_onesf = CF[:, 128:256]
    c_tric = CF[:, 256:384]
    c_decm = CF[:, 384:896].rearrange("p (h c) -> p h c", h=4)
    c_xi = CF[:, 896:1408].rearrange("p (h c) -> p h c", h=4)
    c_zeta = CF[:, 1408:1412]
    c_kdec = CF[:, 1412:1476].rearrange("p (j h) -> p j h", j=16)
    c_fl = CF[:, 1476:1480]
    c_gn = CF[:, 1480:1584]
    c_rnw = CF[:, 1584:1600]
    c_bf = CF[:, 1600:1632]
    c_tri = CF[:, 1632:1760]
    c_ma = CF[:, 1760:1888]
    c_sel2 = CF[:, 1888:2144].rearrange("p (a m) -> p a m", a=2)
    IDB = rs(128); ONB = rs(128); TRIB = rs(128); MAB = rs(128)
    SEL2B = rs(256).rearrange("p (a m) -> p a m", a=2)
    bIDB, bONB, bTRIB, bMAB, bSEL = Buf("idb"), Buf("onb"), Buf("trib"), Buf("mab"), Buf("sel")
    WFF = rs(64)
    bWFF = Buf("wff")

    S.op("sp", lambda e: e.dma_start(out=CF, in_=c_f32), writes=[bCF], dma=bCF)
    for (dst, src, b) in ((IDB, c_identf, bIDB), (ONB, c_onesf, bONB), (TRIB, c_tri, bTRIB), (MAB, c_ma, bMAB)):
        S.op("dve", lambda e, dst=dst, src=src: e.tensor_copy(out=dst, in_=src), reads=[bCF], writes=[b])
    S.op("dve", lambda e: e.tensor_copy(out=SEL2B, in_=c_sel2), reads=[bCF], writes=[bSEL])
    xv = xT_in.rearrange("(c p) t -> p c t", p=128)
    bx = Buf("xload")
    for c in range(8):
        S.op("sp", lambda e, c=c: e.dma_start(out=XT[:, c, :], in_=xv[:, c, :]), writes=XTb[c], dma=bx)

    psbig_b = [Buf(f"psbig{i}") for i in range(3)]
    psbig_i = [0]

    def psbig():
        i = psbig_i[0] % 3
        psbig_i[0] += 1
        return PSB[i], psbig_b[i]

    def mkrot(banks, name):
        bufs = [Buf(f"{name}{i}") for i in range(len(banks))]
        idx = [0]

        def get():
            i = idx[0] % len(banks)
            idx[0] += 1
            return banks[i][:, 0:128], bufs[i]
        get.bufs = bufs
        return get
    ps4 = mkrot([PSB[3], PSB[4]], "ps4")
    ps5 = mkrot([PSB[5]], "ps5")
    ps6 = mkrot([PSB[6]], "ps6")
    pst = mkrot([PST], "pst")
    ps7_banks = [(PSB[0], psbig_b[0]), (PSB[1], psbig_b[1]), (PSB[2], psbig_b[2]), (PSB[3], ps4.bufs[0]), (PSB[4], ps4.bufs[1]),
                 (PSB[5], ps5.bufs[0]), (PSB[6], ps6.bufs[0])]
    ps7_i = [0]

    def ps7():
        i = ps7_i[0] % 7
        ps7_i[0] += 1
        return ps7_banks[i]

    wstate = {"next_dma": 0, "next_get": 0, "total": nl * TPL}

    def wget(k=1):
        f = wstate["next_get"]
        lim = min(f + 3, wstate["total"])
        while wstate["next_dma"] < lim:
            i = wstate["next_dma"]
            sl = i % 3
            S.op("pool", lambda e, i=i, sl=sl: e.dma_start(out=WSL[sl], in_=wseq[i]), writes=[WSLb[sl]], dma=WSLb[sl])
            wstate["next_dma"] += 1
        wstate["next_get"] += k
        return [(WSL[(f + i) % 3], WSLb[(f + i) % 3]) for i in range(k)]

    def tsl(t):
        return slice(t * 512, (t + 1) * 512)

    def mm(ps, psb, lhsT, rhs, rds, start, stop):
        S.op("pe", lambda e: e.matmul(ps, lhsT=lhsT, rhs=rhs, start=start, stop=stop), reads=rds, writes=[psb])

    def rmsnorm(gcol, final=False):
        sq = carve(O_RE, 4096).rearrange("p (c t) -> p c t", c=8)
        sd = carve(O_RD, 1024, F32)
        rstd = carve(O_RD + 1024, 1024, F32)
        bsq, bsd, brs = Buf("sq"), Buf("sd"), Buf("rstd")
        for t in range(NT):
            xr = [XTb[c][t] for c in range(8)]
            S.op("act", lambda e, t=t: e.activation(out=sq, in_=XT[:, :, tsl(t)], func=AF.Square), reads=xr, writes=[bsq])
            ps, pb = psbig()
            for c in range(8):
                mm(ps[:, :], pb, ONB, sq[:, c, :], [bONB, bsq], c == 0, c == 7)
            S.op("act", lambda e, ps=ps: e.activation(out=sd, in_=ps[:, :], func=AF.Sqrt, scale=1.0 / D, bias=EPS), reads=[pb], writes=[bsd])
            S.op("dve", lambda e: e.reciprocal(out=rstd, in_=sd), reads=[bsd], writes=[brs])
            for c in range(8):
                if final:
                    S.op("dve", lambda e, c=c, t=t: e.scalar_tensor_tensor(out=XT[:, c, tsl(t)], in0=XT[:, c, tsl(t)], scalar=c_gn[:, gcol + c:gcol + c + 1], in1=rstd, op0=ALU.mult, op1=ALU.mult),
                         reads=[XTb[c][t], brs, bCF], writes=[XTb[c][t]])
                else:
                    S.op("dve", lambda e, c=c, t=t: e.scalar_tensor_tensor(out=HT[:, c, tsl(t)], in0=XT[:, c, tsl(t)], scalar=c_gn[:, gcol + c:gcol + c + 1], in1=rstd, op0=ALU.mult, op1=ALU.mult),
                         reads=[XTb[c][t], brs, bCF], writes=[HTb[c][t]])

    def ffn(l, which):
        S.barrier(skip=WSLb)
        rmsnorm((l * 3 + (0 if which == 0 else 2)) * 8)
        S.barrier(skip=WSLb)
        actT, actb = RB, RBb
        sl_t = [carve(O_RC + i * 1024, 1024, F32) for i in range(4)]
        sl_b = [Buf(f"slt{i}") for i in range(4)]
        k = 0
        for gi in range(6):
            nch = 4 if gi < 5 else 2
            for ip in range(nch // 2):
                (w, wb), = wget(1)
                wv = w.rearrange("p (c k n) -> p c k n", c=4, k=8)
                for ci in range(2):
                    lc = ip * 2 + ci
                    for t in range(NT):
                        pa, pab = ps7()
                        pbm, pbb = ps7()
                        for kc in range(8):
                            mm(pa[:, :], pab, wv[:, ci, kc, :], HT[:, kc, tsl(t)], [wb, HTb[kc][t]], kc == 0, kc == 7)
                        for kc in range(8):
                            mm(pbm[:, :], pbb, wv[:, 2 + ci, kc, :], HT[:, kc, tsl(t)], [wb, HTb[kc][t]], kc == 0, kc == 7)
                        st, sb = sl_t[k % 4], sl_b[k % 4]
                        k += 1
                        S.op("act", lambda e, pa=pa, st=st: e.activation(out=st, in_=pa[:, :], func=AF.Silu), reads=[pab], writes=[sb])
                        S.op("dve", lambda e, pbm=pbm, st=st, lc=lc, t=t: e.tensor_tensor(out=actT[:, lc, tsl(t)], in0=pbm[:, :], in1=st, op=ALU.mult),
                             reads=[pbb, sb], writes=[actb[lc][t]])
            (wo, wob), = wget(1)
            wov = wo.rearrange("p (c n) -> p c n", c=4)
            for o in range(8):
                for t in range(NT):
                    ps, pb = ps7()
                    for lc in range(nch):
                        mm(ps[:, :], pb, wov[:, lc, o * 128:(o + 1) * 128], actT[:, lc, tsl(t)], [wob, actb[lc][t]], lc == 0, lc == nch - 1)
                    S.op("dve", lambda e, ps=ps, o=o, t=t: e.scalar_tensor_tensor(out=XT[:, o, tsl(t)], in0=ps[:, :], scalar=0.5, in1=XT[:, o, tsl(t)], op0=ALU.mult, op1=ALU.add),
                         reads=[pb, XTb[o][t]], writes=[XTb[o][t]])

    def proj_fm(wv, wb, cc, t, ps, pb):
        for kc in range(8):
            mm(ps, pb, wv[:, cc, kc, :], HT[:, kc, tsl(t)], [wb, HTb[kc][t]], kc == 0, kc == 7)

    def mixer(l):
        S.barrier(skip=WSLb)
        rmsnorm((l * 3 + 1) * 8)
        S.barrier(skip=WSLb)
        scale = 0.125
        tabs = [[carve(O_RD + (i * 2 + j) * 1024, 1024, F32) for j in range(2)] for i in range(2)]
        tabb = [Buf("tab0"), Buf("tab1")]
        t12 = [[carve(O_RD + 4096 + (i * 2 + j) * 1024, 1024, F32) for j in range(2)] for i in range(2)]
        t12b = [[Buf(f"t12_{i}{j}") for j in range(2)] for i in range(2)]
        stg = [carve(O_RE + i * 512, 512) for i in range(2)]
        stgb = [Buf("stg0"), Buf("stg1")]
        kst = [[carve(O_RE + 1024 + (i * 2 + j) * 128, 128) for j in range(2)] for i in range(2)]
        kstb = [[Buf(f"kst{i}{j}") for j in range(2)] for i in range(2)]
        qsq = carve(O_RE + 1536, 512)
        bqsq = Buf("qsq")
        sm = mixer.small
        krT, krb = RB, RBb
        qxiT, qxb = RC, RCb
        S.op("pool", lambda e: e.memset(sm["kmax"], 0.0), writes=[sm["bkmax"]])

        def norm2max(ps, pb, cc, t, acc, accb):
            S.op("act", lambda e: e.activation(out=qsq, in_=ps, func=AF.Square), reads=[pb], writes=[bqsq])
            for hh in range(2):
                p2, p2b = psbig()
                mm(p2[:, :], p2b, SEL2B[:, hh, :], qsq, [bSEL, bqsq], True, True)
                S.op("dve", lambda e, p2=p2: e.reduce_max(out=sm["red"], in_=p2[:, :], axis=AX.X), reads=[p2b], writes=[sm["bred"]])
                col = cc * 2 + hh
                S.op("dve", lambda e, col=col: e.tensor_tensor(out=acc[:, col:col + 1], in0=acc[:, col:col + 1], in1=sm["red"], op=ALU.max),
                     reads=[sm["bred"], accb], writes=[accb])

        (w, wb), = wget(1)
        wv = w.rearrange("p (c k n) -> p c k n", c=4, k=8)
        k = 0
        for cc in range(4):
            for t in range(NT):
                ps, pb = psbig()
                proj_fm(wv, wb, cc, t, ps[:, :], pb)
                st, sb = stg[k % 2], stgb[k % 2]
                k += 1
                S.op("act", lambda e, ps=ps, st=st: e.activation(out=st, in_=ps[:, :], func=AF.Copy), reads=[pb], writes=[sb])
                S.op("sp", lambda e, st=st, cc=cc, t=t: e.dma_start(out=exk[:, cc * T + t * 512: cc * T + (t + 1) * 512], in_=st),
                     reads=[sb], writes=[pc_(f"fk{cc}_{t}")], dma=sb)
                norm2max(ps[:, :], pb, cc, t, sm["kmax"], sm["bkmax"])
        rg_ = [[0, 1], [2, 3], [4, 5], [6, 7]]
        S.op("pool", lambda e: e.collective_compute("AllGather", ALU.bypass, replica_groups=rg_, ins=[EXK.ap().opt()], outs=[GXK.ap().opt()]),
             reads=[b for n, b in pieces.items() if n.startswith("fk")] + [bGXF], writes=[bGXK], dma=bGXK, cc=True)
        if stop == "m2a":
            return
        (w, wb), = wget(1)
        wv = w.rearrange("p (k n) -> p k n", k=8)
        exv_v = exv.rearrange("p (h j e) -> p h j e", h=8, j=16)
        for blk in range(NB):
            t = blk // 4
            ps, pb = psbig()
            for kc in range(8):
                mm(ps[:, :], pb, HT[:, kc, blk * 128:(blk + 1) * 128], wv[:, kc, :], [wb, HTb[kc][t]], kc == 0, kc == 7)
            st, sb = stg[blk % 2], stgb[blk % 2]
            S.op("act", lambda e, ps=ps, st=st: e.activation(out=st, in_=ps[:, :], func=AF.Copy), reads=[pb], writes=[sb])
            S.op("sp", lambda e, st=st, blk=blk: e.dma_start(out=exv_v[:, :, blk, :], in_=st.rearrange("p (h e) -> p h e", h=8)), reads=[sb], writes=[pc_(f"fv{blk}")], dma=sb)
        S.op("pool", lambda e: e.collective_compute("AllGather", ALU.bypass, replica_groups=rg_, ins=[EXV.ap().opt()], outs=[GXV.ap().opt()]),
             reads=[b for n, b in pieces.items() if n.startswith("fv")] + [bGXK], writes=[bGXV], dma=bGXV, cc=True)
        if stop == "m2b":
            return
        (w, wb), = wget(1)
        wv = w.rearrange("p (k n) -> p k n", k=8)
        rvall = carve(O_RC, 8192).rearrange("p (j n) -> p j n", j=16)
        lrv_v = lrv.rearrange("p (h j d) -> p h j d", h=4, j=16)
        for blk in range(NB):
            t = blk // 4
            ps, pb = psbig()
            for kc in range(8):
                mm(ps[:, :], pb, HT[:, kc, blk * 128:(blk + 1) * 128], wv[:, kc, :], [wb, HTb[kc][t]], kc == 0, kc == 7)
            rb_ = RCb[blk // 4][blk % 4]
            S.op("act", lambda e, ps=ps, blk=blk: e.activation(out=rvall[:, blk, :], in_=ps[:, :], func=AF.Copy), reads=[pb], writes=[rb_])
            S.op("sp", lambda e, blk=blk: e.dma_start(out=lrv_v[:, :, blk, :], in_=rvall[:, blk, :].rearrange("p (h d) -> p h d", h=4)), reads=[rb_], writes=[pc_(f"rv{blk}")], dma=rb_)
        if stop == "m2d":
            return
        (w, wb), (w2, wb2) = wget(2)
        wv = w.rearrange("p (c k n) -> p c k n", c=4, k=8)
        wv2 = w2.rearrange("p (c k n) -> p c k n", c=4, k=8)

        def rope_pass(tab_c, tab_s, dst, dstb, post=None):
            kk = 0
            for t in range(NT):
                tb = t % 2
                S.op("sp", lambda e, t=t, tb=tb: e.dma_start(out=tabs[tb][0], in_=c_rope[tab_c][:, tsl(t)]), writes=[tabb[tb]], dma=tabb[tb])
                S.op("sp", lambda e, t=t, tb=tb: e.dma_start(out=tabs[tb][1], in_=c_rope[tab_s][:, tsl(t)]), writes=[tabb[tb]], dma=tabb[tb])
                for h in range(4):
                    p1, p1b = psbig()
                    p2, p2b = psbig()
                    proj_fm(wv, wb, h, t, p1[:, :], p1b)
                    proj_fm(wv2, wb2, h, t, p2[:, :], p2b)
                    i = kk % 2
                    kk += 1
                    S.op("dve", lambda e, p1=p1, i=i, tb=tb: e.tensor_tensor(out=t12[i][0], in0=p1[:, :], in1=tabs[tb][0], op=ALU.mult), reads=[p1b, tabb[tb]], writes=[t12b[i][0]])
                    S.op("dve", lambda e, p2=p2, i=i, tb=tb: e.tensor_tensor(out=t12[i][1], in0=p2[:, :], in1=tabs[tb][1], op=ALU.mult), reads=[p2b, tabb[tb]], writes=[t12b[i][1]])
                    if post is None:
                        S.op("pool", lambda e, i=i, h=h, t=t: e.tensor_tensor(out=dst[:, h, tsl(t)], in0=t12[i][0], in1=t12[i][1], op=ALU.add),
                             reads=[t12b[i][0], t12b[i][1]], writes=[dstb[h][t]])
                    else:
                        S.op("pool", lambda e, i=i: e.tensor_tensor(out=t12[i][0], in0=t12[i][0], in1=t12[i][1], op=ALU.add),
                             reads=[t12b[i][0], t12b[i][1]], writes=[t12b[i][0]])
                        post(i, h, t)
        rope_pass(2, 3, krT, krb)
        lkz_v = lkz.rearrange("p (h j d) -> p h j d", h=4, j=16)
        XF, bXF = sm["XF"], sm["bXF"]
        kk = 0
        for h in range(4):
            pfin, pfinb = ps5()
            for blk in range(NB):
                t = blk // 4
                pt, ptb = pst()
                S.op("pe", lambda e, pt=pt, h=h, blk=blk: e.transpose(pt, krT[:, h, blk * 128:(blk + 1) * 128], IDB), reads=[krb[h][t], bIDB], writes=[ptb])
                i = kk % 2
                kk += 1
                S.op("act", lambda e, pt=pt, i=i, h=h: e.activation(out=kst[i][0], in_=pt, func=AF.Copy, scale=c_zeta[:, h:h + 1]), reads=[ptb, bCF], writes=[kstb[i][0]])
                S.op("dve", lambda e, pt=pt, i=i, h=h, blk=blk: e.tensor_scalar(out=kst[i][1], in0=pt, scalar1=c_kdec[:, blk, h:h + 1], scalar2=None, op0=ALU.mult), reads=[ptb, bCF], writes=[kstb[i][1]])
                S.op("sp", lambda e, i=i, h=h, blk=blk: e.dma_start(out=lkz_v[:, h, blk, :], in_=kst[i][0]), reads=[kstb[i][0]], writes=[pc_(f"kz{h}_{blk}")], dma=kstb[i][0])
                mm(pfin, pfinb, kst[i][1], rvall[:, blk, h * 128:(h + 1) * 128], [kstb[i][1], RCb[blk // 4][blk % 4]], blk == 0, blk == NB - 1)
            S.op("act", lambda e, pfin=pfin, h=h: e.activation(out=XF[:, 264 + h * 128:264 + (h + 1) * 128], in_=pfin, func=AF.Copy), reads=[pfinb], writes=[bXF])
        if stop == "m2c":
            return
        S.op("pool", lambda e: e.dma_start(out=WFF, in_=ffw[l]), writes=[bWFF], dma=bWFF)
        wffv = WFF.rearrange("p (k n) -> p k n", k=8)
        pl, plb = psbig()
        for blk in range(NB):
            t = blk // 4
            for kc in range(8):
                mm(pl[:, blk * 8:(blk + 1) * 8], plb, HT[:, kc, blk * 128:(blk + 1) * 128], wffv[:, kc, :], [bWFF, HTb[kc][t]], kc == 0, kc == 7)
        LF, bLF = sm["LF"], sm["bLF"]
        LF3 = LF.rearrange("p (j h) -> p j h", j=16)
        bfb = c_bf[:, l * 8:(l + 1) * 8].unsqueeze(1).to_broadcast([128, 16, 8])
        S.op("dve", lambda e: e.tensor_tensor(out=LF3, in0=pl[:, 0:128].rearrange("p (j h) -> p j h", j=16), in1=bfb, op=ALU.add), reads=[plb, bCF], writes=[bLF])
        S.op("act", lambda e: e.activation(out=LF, in_=LF, func=AF.Exp, scale=-1.0), reads=[bLF], writes=[bLF])
        S.op("act", lambda e: e.activation(out=LF, in_=LF, func=AF.Ln, bias=1.0), reads=[bLF], writes=[bLF])
        S.op("dve", lambda e: e.tensor_scalar(out=LF, in0=LF, scalar1=-1.0, scalar2=None, op0=ALU.mult), reads=[bLF], writes=[bLF])
        RUN, bRUN = sm["RUN"], sm["bRUN"]
        S.op("dve", lambda e: e.memset(RUN[:, 0:8], 0.0), writes=[bRUN])
        for blk in range(NB):
            S.op("dve", lambda e, blk=blk: e.tensor_tensor(out=RUN[:, (blk + 1) * 8:(blk + 2) * 8], in0=RUN[:, blk * 8:(blk + 1) * 8], in1=LF[:, blk * 8:(blk + 1) * 8], op=ALU.add),
                 reads=[bRUN, bLF], writes=[bRUN])
        pc, pcb = psbig()
        for blk in range(NB):
            mm(pc[:, blk * 8:(blk + 1) * 8], pcb, c_tric, LF[:, blk * 8:(blk + 1) * 8], [bCF, bLF], True, False)
            mm(pc[:, blk * 8:(blk + 1) * 8], pcb, c_onesf, RUN[:, blk * 8:(blk + 1) * 8], [bCF, bRUN], False, True)
        mm(pc[:, 128:136], pcb, c_onesf, RUN[:, 128:136], [bCF, bRUN], True, True)
        CUM, bCUM = sm["CUM"], sm["bCUM"]
        S.op("act", lambda e: e.activation(out=CUM, in_=pc[:, 0:136], func=AF.Copy), reads=[pcb], writes=[bCUM])
        S.op("dve", lambda e: e.tensor_scalar(out=XF[:, 0:128], in0=CUM[:, 0:128], scalar1=-1.0, scalar2=None, op0=ALU.mult), reads=[bCUM], writes=[bXF])
        totb = CUM[:, 128:136].unsqueeze(1).to_broadcast([128, 16, 8])
        S.op("dve", lambda e: e.tensor_tensor(out=XF[:, 128:256].rearrange("p (j h) -> p j h", j=16), in0=totb, in1=CUM[:, 0:128].rearrange("p (j h) -> p j h", j=16), op=ALU.subtract),
             reads=[bCUM], writes=[bXF])
        S.op("dve", lambda e: e.tensor_copy(out=XF[:, 256:264], in_=sm["kmax"]), reads=[sm["bkmax"]], writes=[bXF])
        S.op("sp", lambda e: e.dma_start(out=exf[:, 0:1024], in_=XF), reads=[bXF], writes=[bEXF], dma=bXF)
        if stop == "m2e":
            return
        S.op("pool", lambda e: e.collective_compute("AllGather", ALU.bypass, replica_groups=rg_, ins=[EXF.ap().opt()], outs=[GXF.ap().opt()]),
             reads=[bEXF, bGXV], writes=[bGXF], dma=bGXF, cc=True)
        if stop == "m2f":
            return
        (w, wb), (w2, wb2) = wget(2)
        wv = w.rearrange("p (c k n) -> p c k n", c=4, k=8)
        wv2 = w2.rearrange("p (c k n) -> p c k n", c=4, k=8)

        def qpost(i, h, t):
            xib = c_xi[:, h, :].unsqueeze(1).to_broadcast([128, 4, 128])
            S.op("dve", lambda e: e.tensor_tensor(out=qxiT[:, h, tsl(t)].rearrange("p (j c) -> p j c", j=4), in0=t12[i][0].rearrange("p (j c) -> p j c", j=4), in1=xib, op=ALU.mult),
                 reads=[t12b[i][0], bCF], writes=[qxb[h][t]])
        rope_pass(0, 1, None, None, post=qpost)
        S.barrier(skip=WSLb)

        if stop == "m3":
            return
        (wg, wgb), = wget(1)
        wgv = wg.rearrange("p (c k n) -> p c k n", c=4, k=8)
        kz = carve(O_RD, 2048).rearrange("p (j d) -> p j d", j=16)
        rv = carve(O_RD + 2048, 2048).rearrange("p (j d) -> p j d", j=16)
        retT = [carve(O_RD + 4096 + i * 1024, 1024, F32) for i in range(2)]
        bkz, brv = Buf("kz"), Buf("rv")
        G0, G1, bG = sm["G0"], sm["G1"], sm["bG"]
        S.op("sp", lambda e: e.dma_start(out=G0, in_=gxf[0:128, 0:776]), reads=[bGXF], writes=[bG], dma=bG)
        S.op("sp", lambda e: e.dma_start(out=G1, in_=gxf[128:256, 0:264]), reads=[bGXF], writes=[bG], dma=bG)
        bret = [Buf("ret0"), Buf("ret1")]
        rsq = carve(O_RE, 512)
        rsd = carve(O_RE + 512, 1024, F32)
        rrs = carve(O_RE + 1536, 1024, F32)
        rsg = carve(O_RE + 2560, 1024, F32)
        brsq, brsd, brrs, brsg = Buf("rsq"), Buf("rsd"), Buf("rrs"), Buf("rsg")
        Spf, bSpf = sm["Spf"], sm["bSpf"]
        Pst, bPst = sm["Pst"], sm["bPst"]
        Suse, bSuse = sm["Suse"], sm["bSuse"]
        PTr, bPTr = sm["PTr"], sm["bPTr"]
        YrT, yrb = RC, RCb
        for h in range(4):
            g_c = GAMMA[h] ** 128
            S.op("sp", lambda e, h=h: e.dma_start(out=kz, in_=lkz_v[:, h, :, :]), reads=[pc_(f"kz{h}_{b_}") for b_ in range(NB)], writes=[bkz], dma=bkz)
            S.op("sp", lambda e, h=h: e.dma_start(out=rv, in_=lrv_v[:, h, :, :]), reads=[pc_(f"rv{b_}") for b_ in range(NB)], writes=[brv], dma=brv)
            S.op("dve", lambda e, h=h: e.tensor_scalar(out=Spf, in0=G0[:, 264 + h * 128:264 + (h + 1) * 128], scalar1=c_fl[:, 0:1], scalar2=None, op0=ALU.mult), reads=[bG, bCF], writes=[bSpf])
            S.op("pool", lambda e: e.memset(Pst, 0.0), writes=[bPst])
            for j in range(NB):
                t = j // 4
                jj = j % 4
                csl = slice(j * 128, (j + 1) * 128)
                p1, p1b = ps4()
                mm(p1, p1b, krT[:, h, csl], qxiT[:, h, csl], [krb[h][t], qxb[h][t]], True, True)
                i = j % 2
                S.op("dve", lambda e, p1=p1, i=i, h=h: e.tensor_tensor(out=PTr[i], in0=p1, in1=c_decm[:, h, :], op=ALU.mult), reads=[p1b, bCF], writes=[bPTr[i]])
                S.op("dve", lambda e, i=i, j=j, g_c=g_c: e.scalar_tensor_tensor(out=Suse[i], in0=Spf, scalar=float(g_c ** j), in1=Pst, op0=ALU.mult, op1=ALU.add),
                     reads=[bSpf, bPst], writes=[bSuse[i]])
                po, pob = ps5()
                mm(po, pob, rv[:, j, :], PTr[i], [brv, bPTr[i]], True, False)
                mm(po, pob, Suse[i], qxiT[:, h, csl], [bSuse[i], qxb[h][t]], False, True)
                rt, rtb = retT[t % 2], bret[t % 2]
                S.op("act", lambda e, po=po, rt=rt, jj=jj: e.activation(out=rt[:, jj * 128:(jj + 1) * 128], in_=po, func=AF.Copy), reads=[pob], writes=[rtb])
                pk, pkb = ps4()
                mm(pk, pkb, kz[:, j, :], rv[:, j, :], [bkz, brv], True, True)
                S.op("dve", lambda e, pk=pk, g_c=g_c: e.scalar_tensor_tensor(out=Pst, in0=Pst, scalar=float(g_c), in1=pk, op0=ALU.mult, op1=ALU.add),
                     reads=[pkb, bPst], writes=[bPst])
                if jj == 3:
                    S.op("act", lambda e, rt=rt: e.activation(out=rsq, in_=rt, func=AF.Square), reads=[rtb], writes=[brsq])
                    pn, pnb = psbig()
                    mm(pn[:, :], pnb, ONB, rsq, [bONB, brsq], True, True)
                    S.op("act", lambda e, pn=pn: e.activation(out=rsd, in_=pn[:, :], func=AF.Sqrt, scale=1.0 / 128, bias=EPS), reads=[pnb], writes=[brsd])
                    S.op("dve", lambda e: e.reciprocal(out=rrs, in_=rsd), reads=[brsd], writes=[brrs])
                    pg, pgb = psbig()
                    proj_fm(wgv, wgb, h, t, pg[:, :], pgb)
                    S.op("act", lambda e, pg=pg: e.activation(out=rsg, in_=pg[:, :], func=AF.Silu), reads=[pgb], writes=[brsg])
                    S.op("dve", lambda e, rt=rt, h=h: e.scalar_tensor_tensor(out=rsd, in0=rt, scalar=c_rnw[:, l * 4 + h:l * 4 + h + 1], in1=rrs, op0=ALU.mult, op1=ALU.mult),
                         reads=[rtb, brrs, bCF, brsd], writes=[brsd])
                    S.op("dve", lambda e, h=h, t=t: e.tensor_tensor(out=YrT[:, h, tsl(t)], in0=rsd, in1=rsg, op=ALU.mult), reads=[brsd, brsg], writes=[yrb[h][t]])
        S.barrier(skip=WSLb)

        if stop == "m4":
            return
        (wq, wqb), = wget(1)
        wqv = wq.rearrange("p (c k n) -> p c k n", c=4, k=8)
        KT = carve(O_RD, 4096)
        VA = [carve(O_RD + 4096 + i * 2112, 2112).rearrange("p (j e) -> p j e", j=32) for i in range(2)]
        fqT = carve(O_RE, 2048)
        qsq2 = carve(O_RE + 2048, 512)
        bKT, bVA, bfq, bqs2 = Buf("KT"), [Buf("va0"), Buf("va1")], [Buf(f"fq{t}") for t in range(NT)], Buf("qs2")
        for i in range(2):
            S.op("pool", lambda e, i=i: e.memset(VA[i][:, :, 64:66], 1.0), writes=[bVA[i]])
        NA, NAG, NBt, bNT = sm["NA"], sm["NAG"], sm["NBt"], sm["bNT"]
        S.op("dve", lambda e: e.tensor_scalar(out=NA, in0=G0[:, 0:128], scalar1=c_fl[:, 1:2], scalar2=None, op0=ALU.mult), reads=[bG, bCF], writes=[bNT])
        S.op("dve", lambda e: e.scalar_tensor_tensor(out=NA, in0=G0[:, 128:256], scalar=c_fl[:, 0:1], in1=NA, op0=ALU.mult, op1=ALU.add), reads=[bG, bCF, bNT], writes=[bNT])
        S.op("dve", lambda e: e.tensor_scalar(out=NAG, in0=G0[:, 128:256], scalar1=c_fl[:, 0:1], scalar2=c_fl[:, 2:3], op0=ALU.mult, op1=ALU.add), reads=[bG, bCF], writes=[bNT])
        S.op("dve", lambda e: e.tensor_scalar(out=NBt, in0=G1[:, 0:128], scalar1=c_fl[:, 0:1], scalar2=c_fl[:, 2:3], op0=ALU.mult, op1=ALU.add), reads=[bG, bCF], writes=[bNT])
        KM, bKM = sm["KM"], sm["bKM"]
        S.op("dve", lambda e: e.tensor_tensor(out=KM, in0=G0[:, 256:264], in1=G1[:, 256:264], op=ALU.max), reads=[bG], writes=[bKM])
        NA3 = NA.rearrange("p (j h) -> p j h", j=16)
        NAG3 = NAG.rearrange("p (j h) -> p j h", j=16)
        NB3 = NBt.rearrange("p (j h) -> p j h", j=16)
        CUM3 = CUM[:, 0:128].rearrange("p (j h) -> p j h", j=16)
        gxb_fk = [gxk[r * 128:(r + 1) * 128, :].rearrange("p (c t) -> p c t", c=4) for r in range(2)]
        gxb_fv = [gxv[r * 128:(r + 1) * 128, :].rearrange("p (h j e) -> p h j e", h=8, j=16) for r in range(2)]
        foxT, foxb = RB, RBb
        QM, bQM = sm["QM"], sm["bQM"]
        MC, bMC = sm["MC"], sm["bMC"]
        CM, bCM = sm["CM"], sm["bCM"]
        DG, bDG = sm["DG"], sm["bDG"]
        BQ, bBQ = sm["BQ"], sm["bBQ"]
        TT, bTT = sm["TT"], sm["bTT"]
        PT, bPT = sm["PT"], sm["bPT"]
        FT, bFT = sm["FT"], sm["bFT"]
        REC, bREC = sm["REC"], sm["bREC"]
        TTl = [carve(O_RE + 2560 + i * 256, 256, F32) for i in range(6)]
        _ptb = [sm["TT"][i].bitcast(BF16) for i in range(3)]
        PTl = [_ptb[i // 2][:, (i % 2) * 128:(i % 2 + 1) * 128] for i in range(6)]
        bTTl = [Buf(f"ttl{i}") for i in range(6)]
        bPTl = [Buf(f"ptl{i}") for i in range(6)]
        psA = [(PSB[0], psbig_b[0]), (PSB[1], psbig_b[1]), (PSB[2], psbig_b[2]), (PSB[3], ps4.bufs[0]), (PSB[4], ps4.bufs[1])]
        PBQ = PST[:, 0:256].bitcast(F32)
        psa_i = [0]
        poA = [(PSB[5], ps5.bufs[0]), (PSB[6], ps6.bufs[0])]
        for hp in range(4):
            for r in range(2):
                S.op("sp", lambda e, hp=hp, r=r: e.dma_start(out=KT[:, r * T:(r + 1) * T], in_=gxb_fk[r][:, hp, :]), reads=[bGXK], writes=[bKT], dma=bKT)
                for hh in range(2):
                    S.op("sp", lambda e, hp=hp, r=r, hh=hh: e.dma_start(out=VA[hh][:, r * 16:(r + 1) * 16, 0:64], in_=gxb_fv[r][:, hp * 2 + hh, :, :]), reads=[bGXV], writes=[bVA[hh]], dma=bVA[hh])
            S.op("pool", lambda e: e.memset(QM, 0.0), writes=[bQM])
            for t in range(NT):
                ps, pb = psbig()
                proj_fm(wqv, wqb, hp, t, ps[:, :], pb)
                S.op("act", lambda e, ps=ps, t=t: e.activation(out=fqT[:, tsl(t)], in_=ps[:, :], func=AF.Copy), reads=[pb], writes=[bfq[t]])
                S.op("act", lambda e, ps=ps: e.activation(out=qsq2, in_=ps[:, :], func=AF.Square), reads=[pb], writes=[bqs2])
                for hh in range(2):
                    p2, p2b = psbig()
                    mm(p2[:, :], p2b, SEL2B[:, hh, :], qsq2, [bSEL, bqs2], True, True)
                    S.op("dve", lambda e, p2=p2: e.reduce_max(out=sm["red"], in_=p2[:, :], axis=AX.X), reads=[p2b], writes=[sm["bred"]])
                    S.op("dve", lambda e, hh=hh: e.tensor_tensor(out=QM[:, hh:hh + 1], in0=QM[:, hh:hh + 1], in1=sm["red"], op=ALU.max), reads=[sm["bred"], bQM], writes=[bQM])
            S.op("dve", lambda e, hp=hp: e.tensor_tensor(out=MC, in0=QM, in1=KM[:, hp * 2:hp * 2 + 2], op=ALU.mult), reads=[bQM, bKM], writes=[bMC])
            S.op("act", lambda e: e.activation(out=MC, in_=MC, func=AF.Sqrt, scale=(1.05 * scale) ** 2), reads=[bMC], writes=[bMC])
            for hh in range(2):
                h = hp * 2 + hh
                S.op("dve", lambda e, h=h, hh=hh: e.tensor_scalar(out=CM[:, hh * 16:(hh + 1) * 16], in0=CUM3[:, :, h], scalar1=MC[:, hh:hh + 1], scalar2=None, op0=ALU.subtract),
                     reads=[bCUM, bMC], writes=[bCM])
            units = []
            for j in range(NB):
                for hh in range(2):
                    steps = [(0, g) for g in range(16)] + [(1, g) for g in range(j + 1)]
                    for si, (half, g) in enumerate(steps):
                        units.append({"j": j, "hh": hh, "si": si, "half": half, "g": g, "n": len(steps)})
            LA = 5
            grp = {}

            def emit_S(u, hp=hp):
                j, hh = u["j"], u["hh"]
                rows = slice(hh * 64, (hh + 1) * 64)
                qsl = slice(j * 128, (j + 1) * 128)
                if u["si"] == 0:
                    S.op("dve", lambda e: e.tensor_scalar(out=DG, in0=c_identf, scalar1=CM[:, hh * 16 + j:hh * 16 + j + 1], scalar2=None, op0=ALU.mult), reads=[bCF, bCM], writes=[bDG])
                    pbq, pbqb = PBQ, pst.bufs[0]
                    mm(pbq[:, 0:128], pbqb, c_onesf, DG, [bCF, bDG], True, True)
                    bi = (j * 2 + hh) % 2
                    S.op("dve", lambda e: e.tensor_copy(out=BQ[bi], in_=pbq[:, 0:128]), reads=[pbqb], writes=[bBQ[bi]])
                    grp[(j, hh)] = {"bi": bi, "po": poA[(j * 2 + hh) % 2]}
                k_ = psa_i[0] % 5
                psa_i[0] += 1
                p1, p1b = psA[k_]
                ksl = slice(u["half"] * T + u["g"] * 128, u["half"] * T + (u["g"] + 1) * 128)
                mm(p1[:, 0:128], p1b, KT[rows, ksl], fqT[rows, qsl], [bKT, bfq[j // 4]], True, True)
                u["p1"] = (p1, p1b)

            def emit_rest(u, ui, hp=hp):
                j, hh, si, half, g = u["j"], u["hh"], u["si"], u["half"], u["g"]
                h = hp * 2 + hh
                G = grp[(j, hh)]
                bi = G["bi"]
                po, pob = G["po"]
                p1, p1b = u["p1"]
                i = ui % 6
                S.op("dve", lambda e: e.scalar_tensor_tensor(out=TTl[i], in0=p1[:, 0:128], scalar=scale, in1=BQ[bi], op0=ALU.mult, op1=ALU.add),
                     reads=[p1b, bBQ[bi]], writes=[bTTl[i]])
                if half == 0:
                    tab = NA3 if g <= j else NAG3
                else:
                    tab = NB3
                S.op("act", lambda e: e.activation(out=PTl[i], in_=TTl[i], func=AF.Exp, bias=tab[:, g, h:h + 1]), reads=[bTTl[i], bNT], writes=[bPTl[i]])
                if g == j:
                    msk, mb = (MAB, bMAB) if half == 0 else (TRIB, bTRIB)
                    S.op("pool", lambda e: e.tensor_tensor(out=PTl[i], in0=PTl[i], in1=msk, op=ALU.mult), reads=[bPTl[i], mb], writes=[bPTl[i]])
                mm(po[:, 0:66], pob, PTl[i], VA[hh][:, half * 16 + g, :], [bPTl[i], bVA[hh]], si == 0, si == u["n"] - 1)
                if si == u["n"] - 1:
                    fi = j % 2
                    S.op("dve", lambda e: e.reciprocal(out=REC, in_=po[:, 64:65]), reads=[pob], writes=[bREC])
                    S.op("act", lambda e: e.activation(out=FT[fi][:, hh * 64:(hh + 1) * 64], in_=po[:, 0:64], func=AF.Copy, scale=REC), reads=[pob, bREC], writes=[bFT[fi]])
                    if hh == 1:
                        qsl = slice(j * 128, (j + 1) * 128)
                        pt, ptb = pst()
                        S.op("pe", lambda e: e.transpose(pt, FT[fi], IDB), reads=[bFT[fi], bIDB], writes=[ptb])
                        S.op("dve", lambda e: e.tensor_copy(out=foxT[:, hp, qsl], in_=pt), reads=[ptb], writes=[foxb[hp][j // 4]])

            for idx in range(len(units) + LA):
                if idx - LA >= 0:
                    emit_rest(units[idx - LA], idx - LA)
                if idx < len(units):
                    emit_S(units[idx])
        S.barrier(skip=WSLb)

        if stop == "m5":
            return
        mg = carve(O_RD, 8192).rearrange("p (c t) -> p c t", c=4)
        mgb = [[Buf(f"mg{c}_{t}") for t in range(NT)] for c in range(4)]
        tm = [carve(O_RE + i * 1024, 1024, F32) for i in range(4)]
        tmb = [Buf(f"tm{i}") for i in range(4)]
        halves = []
        for oh in range(2):
            (wo, wob), (wr, wrb), (wf, wfb) = wget(3)
            halves.append(None)
            wov = wo.rearrange("p (a k n) -> p a k n", a=2, k=4)
            wrv = wr.rearrange("p (c k n) -> p c k n", c=4, k=8)
            wfv = wf.rearrange("p (c k n) -> p c k n", c=4, k=8)
            for oc in range(4):
                for t in range(NT):
                    pyr, pyrb = ps7()
                    for kc in range(4):
                        mm(pyr[:, :], pyrb, wov[:, 0, kc, oc * 128:(oc + 1) * 128], YrT[:, kc, tsl(t)], [wob, yrb[kc][t]], kc == 0, kc == 3)
                    pgr, pgrb = ps7()
                    proj_fm(wrv, wrb, oc, t, pgr[:, :], pgrb)
                    S.op("act", lambda e, pgr=pgr: e.activation(out=tm[0], in_=pgr[:, :], func=AF.Sigmoid), reads=[pgrb], writes=[tmb[0]])
                    S.op("dve", lambda e, pyr=pyr: e.tensor_tensor(out=tm[1], in0=pyr[:, :], in1=tm[0], op=ALU.mult), reads=[pyrb, tmb[0]], writes=[tmb[1]])
                    pyf, pyfb = ps7()
                    for kc in range(4):
                        mm(pyf[:, :], pyfb, wov[:, 1, kc, oc * 128:(oc + 1) * 128], foxT[:, kc, tsl(t)], [wob, foxb[kc][t]], kc == 0, kc == 3)
                    pgf, pgfb = ps7()
                    proj_fm(wfv, wfb, oc, t, pgf[:, :], pgfb)
                    S.op("act", lambda e, pgf=pgf: e.activation(out=tm[2], in_=pgf[:, :], func=AF.Sigmoid), reads=[pgfb], writes=[tmb[2]])
                    S.op("dve", lambda e, pyf=pyf: e.tensor_tensor(out=tm[3], in0=pyf[:, :], in1=tm[2], op=ALU.mult), reads=[pyfb, tmb[2]], writes=[tmb[3]])
                    S.op("pool", lambda e, oc=oc, t=t: e.tensor_tensor(out=mg[:, oc, tsl(t)], in0=tm[1], in1=tm[3], op=ALU.add), reads=[tmb[1], tmb[3]], writes=[mgb[oc][t]])
            (ww, wwb), = wget(1)
            wwv = ww.rearrange("p (k n) -> p k n", k=4)
            for o in range(8):
                for t in range(NT):
                    ps, pb = ps7()
                    for kc in range(4):
                        mm(ps[:, :], pb, wwv[:, kc, o * 128:(o + 1) * 128], mg[:, kc, tsl(t)], [wwb, mgb[kc][t]], kc == 0, kc == 3)
                    S.op("dve", lambda e, ps=ps, o=o, t=t: e.tensor_tensor(out=XT[:, o, tsl(t)], in0=ps[:, :], in1=XT[:, o, tsl(t)], op=ALU.add),
                         reads=[pb, XTb[o][t]], writes=[XTb[o][t]])

    sm = {}

    def smt(name, ncols, dt=F32, n=1):
        if n == 1:
            sm[name] = rs(ncols * (2 if dt == F32 else 1), dt)
            sm["b" + name] = Buf(name)
        else:
            sm[name] = [rs(ncols * (2 if dt == F32 else 1), dt) for _ in range(n)]
            sm["b" + name] = [Buf(f"{name}{i}") for i in range(n)]
    smt("kmax", 8); smt("red", 1); smt("LF", 128); smt("RUN", 136); smt("CUM", 136); smt("XF", 1024)
    smt("Spf", 128); smt("Pst", 128); smt("Suse", 128, BF16, 2); smt("PTr", 128, BF16, 2)
    smt("G0", 776); smt("G1", 264)
    sm["NA"] = rs(256, F32); sm["NAG"] = rs(256, F32); sm["NBt"] = rs(256, F32); sm["bNT"] = Buf("nt")
    sm["bG"] = Buf("g01")
    smt("KM", 8); smt("QM", 2); smt("MC", 2); smt("CM", 32); smt("DG", 128)
    smt("BQ", 128, F32, 2); smt("TT", 128, F32, 3); smt("PT", 128, BF16, 3); smt("FT", 128, BF16, 2); smt("REC", 1)
    mixer.small = sm
    S.op("pool", lambda e: e.memset(sm["XF"], 0.0), writes=[sm["bXF"]])

    for l in range(nl):
        ffn(l, 0)
        if stop == "ffn1":
            break
        mixer(l)
        if stop is not None:
            break
        ffn(l, 1)
    S.barrier(skip=WSLb)
    rmsnorm(12 * 8, final=True)
    ov = outT.rearrange("(c p) t -> p c t", p=128)
    bo = Buf("ostore")
    for c in range(8):
        S.op("sp", lambda e, c=c: e.dma_start(out=ov[:, c, :], in_=XT[:, c, :]), reads=XTb[c], dma=bo)
    S.barrier()
    print('nsem', S.nsem, 'nops', S.nops, {e: len(S.lists[e]) for e in S.ENGS})
    S.emit()
    return nc


def _fm(Wc):
    return np.ascontiguousarray(Wc.reshape(8, 128, 4, 128).transpose(1, 2, 0, 3)).reshape(128, 4096)


def _tm(Wc):
    return np.ascontiguousarray(Wc.reshape(8, 128, 512).transpose(1, 0, 2)).reshape(128, 4096)


def _swap_cols(Wc):
    w = Wc.reshape(Wc.shape[0], 4, 2, 64)
    return w[:, :, ::-1, :].reshape(Wc.shape[0], 512)


def _ffn_tiles(w_in, w_out):
    tiles = []
    for gi in range(6):
        nch = 4 if gi < 5 else 2
        for ip in range(nch // 2):
            p = gi * 2 + ip
            cols = np.concatenate([w_in[:, p * 256:(p + 1) * 256], w_in[:, DFF + p * 256:DFF + (p + 1) * 256]], axis=1)
            tiles.append(_fm(cols))
        wo = np.zeros((4, 128, 1024), np.float32)
        wo[:nch] = w_out[gi * 512: gi * 512 + nch * 128].reshape(nch, 128, 1024)
        tiles.append(np.ascontiguousarray(wo.transpose(1, 0, 2)).reshape(128, 4096))
    return tiles


def _mixer_tiles(w_in, w_o_ret, w_o_fox, w_out):
    t = []
    t.append(_fm(w_in[:, 2560:3072]))
    t.append(_tm(w_in[:, 3072:3584]))
    t.append(_tm(w_in[:, 1024:1536]))
    t.append(_fm(w_in[:, 512:1024]))
    t.append(_fm(_swap_cols(w_in[:, 512:1024])))
    t.append(_fm(w_in[:, 0:512]))
    t.append(_fm(_swap_cols(w_in[:, 0:512])))
    t.append(_fm(w_in[:, 1536:2048]))
    t.append(_fm(w_in[:, 2048:2560]))
    for oh in range(2):
        a = np.stack([w_o_ret[:, oh * 512:(oh + 1) * 512].reshape(4, 128, 512), w_o_fox[:, oh * 512:(oh + 1) * 512].reshape(4, 128, 512)], 0)
        t.append(np.ascontiguousarray(a.transpose(2, 0, 1, 3)).reshape(128, 4096))
        t.append(_fm(w_in[:, 3592 + oh * 512:3592 + (oh + 1) * 512]))
        t.append(_fm(w_in[:, 4616 + oh * 512:4616 + (oh + 1) * 512]))
        wo = w_out[oh * 512:(oh + 1) * 512].reshape(4, 128, 1024)
        t.append(np.ascontiguousarray(wo.transpose(1, 0, 2)).reshape(128, 4096))
    return t


def _consts(rank, norm_ffn1, norm_mix, norm_ffn2, norm_final, ret_norm, b_forget):
    cf = np.zeros((128, 2176), np.float32)
    cf[:, 0:128] = np.eye(128, dtype=np.float32)
    cf[:, 128:256] = 1.0
    idx = np.arange(128)
    cf[:, 256:384] = (idx[:, None] <= idx[None, :]).astype(np.float32)
    g = np.array(GAMMA, np.float64)
    lg = np.log(g)
    decm = np.where(idx[None, None, :] >= idx[:, None, None], np.exp(-lg[None, :, None] * (idx[:, None, None] + 1.0)), 0.0)
    cf[:, 384:896] = decm.reshape(128, 512)
    xi = np.exp(lg[:, None] * (idx[None, :] + 1.0))
    cf[:, 896:1408] = np.broadcast_to(xi.reshape(1, 512), (128, 512))
    cf[:, 1408:1412] = np.exp(lg[None, :] * (127.0 - idx[:, None]))
    tt = (np.arange(16)[None, :, None] * 128 + idx[:, None, None]).astype(np.float64)
    cf[:, 1412:1476] = np.exp(lg[None, None, :] * (2047.0 - tt)).reshape(128, 64)
    fl = float(rank)
    cf[:, 1476] = fl
    cf[:, 1477] = 1.0 - fl
    cf[:, 1478] = (1.0 - fl) * -30000.0
    gn = np.zeros((13, 1024), np.float32)
    for l in range(DEPTH):
        gn[l * 3 + 0] = norm_ffn1[l]
        gn[l * 3 + 1] = norm_mix[l]
        gn[l * 3 + 2] = norm_ffn2[l]
    gn[12] = norm_final
    cf[:, 1480:1584] = gn.reshape(13, 8, 128).transpose(2, 0, 1).reshape(128, 104)
    cf[:, 1584:1600] = ret_norm.reshape(4, 4, 128).transpose(2, 0, 1).reshape(128, 16)
    cf[:, 1600:1632] = np.broadcast_to(b_forget.reshape(1, 32), (128, 32))
    tri = (idx[:, None] <= idx[None, :]).astype(np.float32)
    cf[:, 1632:1760] = tri
    cf[:, 1760:1888] = tri if rank == 0 else 1.0
    sel = np.zeros((128, 2, 128), np.float32)
    sel[0:64, 0, :] = 1.0
    sel[64:128, 1, :] = 1.0
    cf[:, 1888:2144] = sel.reshape(128, 256)
    pos = (rank * T + np.arange(T)).astype(np.float32)
    inv = np.power(np.float32(10000.0), -np.arange(0, 128, 2, dtype=np.float32) / np.float32(128)).astype(np.float32)
    ang = (pos[:, None] * inv[None, :]).astype(np.float32)
    cos = np.cos(ang).T.astype(np.float32)
    sin = np.sin(ang).T.astype(np.float32)
    COS = np.concatenate([cos, cos], 0)
    SIN = np.concatenate([-sin, sin], 0)
    ks = np.float32(128 ** -0.5)
    rope = np.stack([COS, SIN, COS * ks, SIN * ks], 0).astype(np.float32)
    return cf, rope


_CACHE = {}


def kernel(x, norm_ffn1, w_ffn1_in, w_ffn1_out, norm_mix, w_in, b_forget, ret_norm,
           w_o_ret, w_o_fox, w_out, norm_ffn2, w_ffn2_in, w_ffn2_out, norm_final, _nl=DEPTH, _stop=None):
    f = lambda a: np.asarray(a, dtype=np.float32)
    x = f(x)
    nl = _nl
    tiles = []
    for l in range(nl):
        tiles += _ffn_tiles(f(w_ffn1_in[l]), f(w_ffn1_out[l]))
        tiles += _mixer_tiles(f(w_in[l]), f(w_o_ret[l]), f(w_o_fox[l]), f(w_out[l]))
        tiles += _ffn_tiles(f(w_ffn2_in[l]), f(w_ffn2_out[l]))
    wseq = np.stack(tiles, 0)
    assert wseq.shape[0] == nl * TPL, wseq.shape
    ffw = np.stack([np.ascontiguousarray(f(w_in[l])[:, 3584:3592].reshape(8, 128, 8).transpose(1, 0, 2)).reshape(128, 64) for l in range(nl)], 0)
    in_maps = []
    for c in range(8):
        b, r = c // 2, c % 2
        cf, rope = _consts(r, f(norm_ffn1), f(norm_mix), f(norm_ffn2), f(norm_final), f(ret_norm), f(b_forget))
        xT = np.ascontiguousarray(x[b, r * T:(r + 1) * T, :].T)
        in_maps.append({"xT": xT, "wseq": wseq, "ffw": ffw, "c_f32": cf, "c_rope": rope})
    if (nl, _stop) not in _CACHE:
        _CACHE[(nl, _stop)] = build_nc(nl, _stop)
    nc = _CACHE[(nl, _stop)]
    res = run_bass_kernel_spmd(nc, in_maps, core_ids=list(range(8)))
    out = np.empty((NBATCH, SEQ, D), np.float32)
    for c in range(8):
        b, r = c // 2, c % 2
        out[b, r * T:(r + 1) * T, :] = res.results[c]["outT"].T
    return out
```
